# Optimizing a Trainium2 kernel written in Bass

```python
import math
import jax
import jax.numpy as jnp
from jax import lax
import numpy as np

D_MODEL = 1024
BATCH = 4
SEQ = 4096
DEPTH = 2

N_BRANCH = 4
W_BRANCH = 256
NORM_EPS = 1e-6

S5_GROUP = 16
S5_GROUPS = W_BRANCH // S5_GROUP
S5_STATE = 64

ATT_HEADS = 4
ATT_DIM = W_BRANCH // ATT_HEADS
IDX_HEADS = 4
IDX_DIM = 32
TOPK_MAX = 256
Q_BLOCK = 128
ROPE_THETA = 10000.0

RG_BLOCKS = 4
RG_BDIM = W_BRANCH // RG_BLOCKS
RG_C = 8.0
CONV_WIDTH = 4

RW_HEADS = 4
RW_DIM = W_BRANCH // RW_HEADS
RW_DECAY_LORA = 32
RW_A_LORA = 32
RW_GATE_LORA = 64
RW_GN_EPS = 64e-5
RW_WIDTHS = (W_BRANCH, W_BRANCH, W_BRANCH, RW_DECAY_LORA, RW_A_LORA, RW_GATE_LORA)
RW_COLS = 3 * W_BRANCH + RW_DECAY_LORA + RW_A_LORA + RW_GATE_LORA

IN_WIDTHS = (W_BRANCH,
             W_BRANCH, W_BRANCH, W_BRANCH,
             IDX_HEADS * IDX_DIM, IDX_DIM, IDX_HEADS,
             W_BRANCH, W_BRANCH,
             RW_COLS)
D_IN = 6 * W_BRANCH + IDX_HEADS * IDX_DIM + IDX_DIM + IDX_HEADS + RW_COLS

N_EXPERTS = 16
N_GROUPS = 4
EXP_PER_GROUP = N_EXPERTS // N_GROUPS
TOP_K = 2
D_EXPERT = 512

kernel_name = 'hybrid_gated_s5_dsa_rglru_rwkv7_moe'


def _split(a, widths):
    offs = []
    o = 0
    for w in widths[:-1]:
        o += w
        offs.append(o)
    return jnp.split(a, offs, axis=-1)


def rmsnorm(x, g):
    xf = x.astype(jnp.float32)
    y = xf * lax.rsqrt(jnp.mean(xf * xf, axis=-1, keepdims=True) + NORM_EPS)
    return (y * g.astype(jnp.float32)).astype(x.dtype)


def rope(x, pos):
    half = x.shape[-1] // 2
    inv_freq = ROPE_THETA ** (-jnp.arange(half, dtype=jnp.float32) / half)
    ang = pos.astype(jnp.float32)[:, :, None, None] * inv_freq
    cos, sin = jnp.cos(ang), jnp.sin(ang)
    xf = x.astype(jnp.float32)
    x1, x2 = xf[..., :half], xf[..., half:]
    return jnp.concatenate([x1 * cos - x2 * sin, x2 * cos + x1 * sin], axis=-1).astype(x.dtype)


def linear_scan(a, b):
    def combine(left, right):
        return right[0] * left[0], right[0] * left[1] + right[1]
    return lax.associative_scan(combine, (a, b), axis=1)[1]


def s5_mixer(u, lam_re, lam_im, log_step, b_re, b_im, c_re, c_im, d_skip, glu_w, glu_b):
    bsz, seq, _ = u.shape
    f32 = jnp.float32
    lam = lax.complex(lam_re.astype(f32), lam_im.astype(f32))
    step = jnp.exp(log_step.astype(f32))[:, None]
    lam_bar = jnp.exp(lam * step)
    b_bar = ((lam_bar - 1.0) / lam)[:, :, None] * lax.complex(b_re.astype(f32), b_im.astype(f32))
    ug = u.astype(f32).reshape(bsz, seq, S5_GROUPS, S5_GROUP)
    bu = jnp.einsum('btgc,gpc->btgp', ug.astype(jnp.complex64), b_bar)
    states = linear_scan(jnp.broadcast_to(lam_bar, bu.shape), bu)
    c_mat = lax.complex(c_re.astype(f32), c_im.astype(f32))
    y = jnp.real(jnp.einsum('btgp,gcp->btgc', states, c_mat)) + d_skip.astype(f32).reshape(S5_GROUPS, S5_GROUP) * ug
    y = jax.nn.gelu(y.reshape(bsz, seq, W_BRANCH)).astype(u.dtype)
    return y * jax.nn.sigmoid(y @ glu_w + glu_b)


def dsa_mixer(q, k, v, iq, ik, iw, pos):
    bsz, seq = q.shape[:2]
    f32 = jnp.float32
    topk = min(TOPK_MAX, seq // 4)
    n_blocks = seq // Q_BLOCK
    q = rope(q, pos) * (ATT_DIM ** -0.5)
    k = rope(k, pos)
    iq = rope(iq, pos)
    ik = rope(ik[:, :, None, :], pos)[:, :, 0].astype(f32)
    key_pos = jnp.arange(seq)
    gather = jax.vmap(lambda arr, ix: arr[ix])

    def blocks(a):
        return a.reshape(bsz, n_blocks, Q_BLOCK, *a.shape[2:]).swapaxes(0, 1)

    def one_block(args):
        qb, iqb, iwb, t0 = args
        q_pos = t0 + jnp.arange(Q_BLOCK)
        causal = key_pos[None, :] <= q_pos[:, None]
        dots = jnp.einsum('bqhd,bsd->bqhs', iqb.astype(f32), ik)
        score = jnp.einsum('bqh,bqhs->bqs', iwb.astype(f32), jax.nn.relu(dots))
        score = jnp.where(causal[None], score, -jnp.inf)
        _, idx = lax.top_k(score, topk)
        k_sel = gather(k, idx).astype(f32)
        v_sel = gather(v, idx).astype(f32)
        logits = jnp.einsum('bqhd,bqkhd->bhqk', qb.astype(f32), k_sel)
        valid = (idx <= q_pos[None, :, None])[:, None]
        p = jax.nn.softmax(jnp.where(valid, logits, -jnp.inf), axis=-1)
        return jnp.einsum('bhqk,bqkhd->bqhd', p, v_sel).astype(qb.dtype)

    out = lax.map(one_block, (blocks(q), blocks(iq), blocks(iw), jnp.arange(n_blocks) * Q_BLOCK))
    return out.swapaxes(0, 1).reshape(bsz, seq, ATT_HEADS * ATT_DIM)


def rglru_mixer(xr, gate, conv_w, conv_b, w_r, b_r, w_i, b_i, lam):
    bsz, seq, width = xr.shape
    xc = lax.conv_general_dilated(xr, conv_w[:, None, :], window_strides=(1,),
                                  padding=[(CONV_WIDTH - 1, 0)],
                                  dimension_numbers=('NWC', 'WIO', 'NWC'),
                                  feature_group_count=width) + conv_b
    xc = xc.astype(jnp.float32)
    xb = xc.reshape(bsz, seq, RG_BLOCKS, RG_BDIM)
    r = jax.nn.sigmoid(jnp.einsum('btnc,ncd->btnd', xb, w_r) + b_r).reshape(bsz, seq, width)
    i = jax.nn.sigmoid(jnp.einsum('btnc,ncd->btnd', xb, w_i) + b_i).reshape(bsz, seq, width)
    log_a = -RG_C * r * jax.nn.softplus(-lam.astype(jnp.float32))
    a = jnp.exp(log_a)
    mult = jnp.sqrt(-jnp.expm1(2.0 * log_a))
    h = linear_scan(a, mult * i * xc)
    return (h * jax.nn.gelu(gate.astype(jnp.float32))).astype(xr.dtype)


def rwkv7_mixer(feat, mu, w0, w_up, a0, a_up, g_up, k_k, k_a, r_k, gn_w, gn_b):
    bsz, seq, _ = feat.shape
    f = feat.astype(jnp.float32)
    prev = jnp.pad(f, ((0, 0), (1, 0), (0, 0)))[:, :-1]
    f = f + (prev - f) * mu
    r, k, v, wd, ad, gd = _split(f, RW_WIDTHS)
    w = -jax.nn.softplus(-(w0 + jnp.tanh(wd) @ w_up)) - 0.5
    decay = jnp.exp(-jnp.exp(w))
    a = jax.nn.sigmoid(a0 + ad @ a_up)
    g = jax.nn.sigmoid(gd) @ g_up

    def heads(t):
        return t.reshape(bsz, seq, RW_HEADS, RW_DIM)

    kk = heads(k * k_k)
    kk = kk / jnp.maximum(jnp.linalg.norm(kk, axis=-1, keepdims=True), 1e-12)
    k = k * (1.0 + (a - 1.0) * k_a)
    r_h, k_h, v_h, a_h, d_h = heads(r), heads(k), heads(v), heads(a), heads(decay)

    def step(state, inp):
        r_t, d_t, k_t, v_t, kk_t, a_t = inp
        s_kk = jnp.einsum('bhvk,bhk->bhv', state, kk_t)
        state = (state * d_t[:, :, None, :]
                 - s_kk[..., None] * (kk_t * a_t)[:, :, None, :]
                 + v_t[..., None] * k_t[:, :, None, :])
        return state, jnp.einsum('bhvk,bhk->bhv', state, r_t)

    xs = tuple(jnp.moveaxis(t, 1, 0) for t in (r_h, d_h, k_h, v_h, kk, a_h))
    _, y = lax.scan(step, jnp.zeros((bsz, RW_HEADS, RW_DIM, RW_DIM), jnp.float32), xs)
    y = jnp.moveaxis(y, 0, 1)
    mean = jnp.mean(y, axis=-1, keepdims=True)
    var = jnp.mean(jnp.square(y - mean), axis=-1, keepdims=True)
    y = ((y - mean) * lax.rsqrt(var + RW_GN_EPS)).reshape(bsz, seq, W_BRANCH) * gn_w + gn_b
    bonus = jnp.sum(r_h * k_h * r_k, axis=-1, keepdims=True) * v_h
    y = y + bonus.reshape(bsz, seq, W_BRANCH)
    return (y * g).astype(feat.dtype)


def moe(h, router_w, router_b, w1, w3, w2):
    bsz, seq, d = h.shape
    hf = h.reshape(bsz * seq, d)
    scores = jax.nn.sigmoid(hf.astype(jnp.float32) @ router_w.astype(jnp.float32))
    biased = scores + router_b.astype(jnp.float32)
    group_score = jnp.sum(lax.top_k(biased.reshape(-1, N_GROUPS, EXP_PER_GROUP), TOP_K)[0], axis=-1)
    best_group = jnp.argmax(group_score, axis=-1)
    in_group = (jnp.arange(N_EXPERTS) // EXP_PER_GROUP)[None, :] == best_group[:, None]
    _, idx = lax.top_k(jnp.where(in_group, biased, -jnp.inf), TOP_K)
    sel = jnp.take_along_axis(scores, idx, axis=-1)
    weights = sel / jnp.sum(sel, axis=-1, keepdims=True)
    combine = jnp.sum(jax.nn.one_hot(idx, N_EXPERTS, dtype=jnp.float32) * weights[..., None], axis=1)

    def add_expert(acc, p):
        e_w1, e_w3, e_w2, e_c = p
        y = (jax.nn.silu(hf @ e_w1) * (hf @ e_w3)) @ e_w2
        return acc + e_c[:, None] * y.astype(jnp.float32), None

    out, _ = lax.scan(add_expert, jnp.zeros((bsz * seq, d), jnp.float32), (w1, w3, w2, combine.T))
    return out.reshape(bsz, seq, d).astype(h.dtype)


def _normal(key, shape, scale):
    return scale * jax.random.normal(key, shape, jnp.float32)


def setup_inputs(seed: int = 0) -> dict:
    key = jax.random.key(seed)
    ks = iter(jax.random.split(key, 48))
    L, D, W = DEPTH, D_MODEL, W_BRANCH
    G, P, C = S5_GROUPS, S5_STATE, S5_GROUP
    inp = {}
    inp['x'] = _normal(next(ks), (BATCH, SEQ, D), 1.0)
    inp['c'] = _normal(next(ks), (BATCH, D), 1.0)
    offset = jax.random.randint(next(ks), (BATCH, 1), 0, 2048)
    inp['positions'] = (offset + jnp.arange(SEQ)[None, :]).astype(jnp.int32)
    inp['ada_w'] = _normal(next(ks), (L, D, 6 * D), 0.5 * D ** -0.5)
    inp['ada_b'] = _normal(next(ks), (L, 6 * D), 0.02)
    inp['mix_norm_g'] = 1.0 + _normal(next(ks), (L, D), 0.02)
    inp['w_in'] = _normal(next(ks), (L, D, D_IN), D ** -0.5)
    inp['gate_w'] = _normal(next(ks), (L, D, N_BRANCH * D), D ** -0.5)
    inp['gate_b'] = _normal(next(ks), (L, N_BRANCH * D), 0.02)
    inp['branch_w'] = _normal(next(ks), (L, N_BRANCH, W, D), W ** -0.5)
    inp['out_w'] = _normal(next(ks), (L, D, D), D ** -0.5)
    inp['s5_lam_re'] = -0.5 + _normal(next(ks), (L, G, P), 0.01)
    inp['s5_lam_im'] = math.pi * jnp.arange(P, dtype=jnp.float32) + _normal(next(ks), (L, G, P), 0.01)
    inp['s5_log_step'] = jax.random.uniform(next(ks), (L, G), jnp.float32, math.log(1e-3), math.log(1e-1))
    inp['s5_b_re'] = _normal(next(ks), (L, G, P, C), (2 * C) ** -0.5)
    inp['s5_b_im'] = _normal(next(ks), (L, G, P, C), (2 * C) ** -0.5)
    inp['s5_c_re'] = _normal(next(ks), (L, G, C, P), (2 * P) ** -0.5)
    inp['s5_c_im'] = _normal(next(ks), (L, G, C, P), (2 * P) ** -0.5)
    inp['s5_d'] = _normal(next(ks), (L, W), 1.0)
    inp['s5_glu_w'] = _normal(next(ks), (L, W, W), W ** -0.5)
    inp['s5_glu_b'] = _normal(next(ks), (L, W), 0.02)
    inp['rg_conv_w'] = _normal(next(ks), (L, CONV_WIDTH, W), CONV_WIDTH ** -0.5)
    inp['rg_conv_b'] = _normal(next(ks), (L, W), 0.02)
    inp['rg_wr'] = _normal(next(ks), (L, RG_BLOCKS, RG_BDIM, RG_BDIM), RG_BDIM ** -0.5)
    inp['rg_br'] = _normal(next(ks), (L, RG_BLOCKS, RG_BDIM), 0.02)
    inp['rg_wi'] = _normal(next(ks), (L, RG_BLOCKS, RG_BDIM, RG_BDIM), RG_BDIM ** -0.5)
    inp['rg_bi'] = _normal(next(ks), (L, RG_BLOCKS, RG_BDIM), 0.02)
    a_pow = jax.random.uniform(next(ks), (L, W), jnp.float32, 0.9, 0.999)
    a_base = a_pow ** (1.0 / RG_C)
    inp['rg_lam'] = jnp.log(a_base) - jnp.log1p(-a_base)
    inp['rw_mu'] = jax.random.uniform(next(ks), (L, RW_COLS), jnp.float32, 0.0, 1.0)
    inp['rw_w0'] = jax.random.uniform(next(ks), (L, W), jnp.float32, -6.0, -1.0)
    inp['rw_w_up'] = _normal(next(ks), (L, RW_DECAY_LORA, W), 0.1 * RW_DECAY_LORA ** -0.5)
    inp['rw_a0'] = _normal(next(ks), (L, W), 0.1)
    inp['rw_a_up'] = _normal(next(ks), (L, RW_A_LORA, W), 0.5 * RW_A_LORA ** -0.5)
    inp['rw_g_up'] = _normal(next(ks), (L, RW_GATE_LORA, W), RW_GATE_LORA ** -0.5)
    inp['rw_k_k'] = 0.85 + _normal(next(ks), (L, W), 0.02)
    inp['rw_k_a'] = 1.0 + _normal(next(ks), (L, W), 0.02)
    inp['rw_r_k'] = _normal(next(ks), (L, RW_HEADS, RW_DIM), 0.1)
    inp['rw_gn_w'] = 1.0 + _normal(next(ks), (L, W), 0.02)
    inp['rw_gn_b'] = _normal(next(ks), (L, W), 0.02)
    inp['ffn_norm_g'] = 1.0 + _normal(next(ks), (L, D), 0.02)
    inp['router_w'] = _normal(next(ks), (D, N_EXPERTS), D ** -0.5)
    inp['router_b'] = _normal(next(ks), (N_EXPERTS,), 0.01)
    inp['exp_w1'] = _normal(next(ks), (L, N_EXPERTS, D, D_EXPERT), D ** -0.5)
    inp['exp_w3'] = _normal(next(ks), (L, N_EXPERTS, D, D_EXPERT), D ** -0.5)
    inp['exp_w2'] = _normal(next(ks), (L, N_EXPERTS, D_EXPERT, D), D_EXPERT ** -0.5)
    inp['final_norm_g'] = 1.0 + _normal(next(ks), (D,), 0.02)
    return inp


def reference(x, c, positions, ada_w, ada_b, mix_norm_g, w_in, gate_w, gate_b, branch_w, out_w,
              s5_lam_re, s5_lam_im, s5_log_step, s5_b_re, s5_b_im, s5_c_re, s5_c_im, s5_d, s5_glu_w, s5_glu_b,
              rg_conv_w, rg_conv_b, rg_wr, rg_br, rg_wi, rg_bi, rg_lam,
              rw_mu, rw_w0, rw_w_up, rw_a0, rw_a_up, rw_g_up, rw_k_k, rw_k_a, rw_r_k, rw_gn_w, rw_gn_b,
              ffn_norm_g, router_w, router_b, exp_w1, exp_w3, exp_w2, final_norm_g):
    bsz, seq, d = x.shape
    c_act = jax.nn.silu(c)
    for l in range(DEPTH):
        mod = (c_act @ ada_w[l] + ada_b[l]).reshape(bsz, 6, 1, d)
        shift1, scale1, gate1, shift2, scale2, gate2 = (mod[:, i] for i in range(6))
        h = rmsnorm(x, mix_norm_g[l]) * (1.0 + scale1) + shift1
        u, q, k, v, iq, ik, iw, rg_x, rg_g, rw_f = _split(h @ w_in[l], IN_WIDTHS)
        o_s5 = s5_mixer(u, s5_lam_re[l], s5_lam_im[l], s5_log_step[l], s5_b_re[l], s5_b_im[l],
                        s5_c_re[l], s5_c_im[l], s5_d[l], s5_glu_w[l], s5_glu_b[l])
        o_dsa = dsa_mixer(q.reshape(bsz, seq, ATT_HEADS, ATT_DIM),
                          k.reshape(bsz, seq, ATT_HEADS, ATT_DIM),
                          v.reshape(bsz, seq, ATT_HEADS, ATT_DIM),
                          iq.reshape(bsz, seq, IDX_HEADS, IDX_DIM), ik, iw, positions)
        o_rg = rglru_mixer(rg_x, rg_g, rg_conv_w[l], rg_conv_b[l], rg_wr[l], rg_br[l],
                           rg_wi[l], rg_bi[l], rg_lam[l])
        o_rw = rwkv7_mixer(rw_f, rw_mu[l], rw_w0[l], rw_w_up[l], rw_a0[l], rw_a_up[l], rw_g_up[l],
                           rw_k_k[l], rw_k_a[l], rw_r_k[l], rw_gn_w[l], rw_gn_b[l])
        branches = jnp.stack([o_s5, o_dsa, o_rg, o_rw], axis=2)
        up = jnp.einsum('btnw,nwd->btnd', branches, branch_w[l])
        gates = jax.nn.sigmoid(h @ gate_w[l] + gate_b[l]).reshape(bsz, seq, N_BRANCH, d)
        mixed = jnp.einsum('btnd,btnd->btd', gates, up) @ out_w[l]
        x = x + gate1 * mixed
        h2 = rmsnorm(x, ffn_norm_g[l]) * (1.0 + scale2) + shift2
        x = x + gate2 * moe(h2, router_w, router_b, exp_w1[l], exp_w3[l], exp_w2[l])
    return rmsnorm(x, final_norm_g)
```

```python
import math
from contextlib import ExitStack

import numpy as np
import concourse.bass as bass
import concourse.mybir as mybir
from concourse.bass_utils import run_bass_kernel_spmd

F32 = mybir.dt.float32
BF16 = mybir.dt.bfloat16
I32 = mybir.dt.int32
AF = mybir.ActivationFunctionType
ALU = mybir.AluOpType

D = 1024
DC = 8
W = 256
D_IN = 2596
NE = 16
DE = 512
EPS = 1e-6
BIG = 1.0e30

C_U, C_Q, C_K, C_V, C_IQ, C_IK, C_IW, C_RGX, C_RGG, C_RW = 0, 256, 512, 768, 1024, 1152, 1184, 1188, 1444, 1700

import os
S5DBG = int(os.environ.get('S5DBG', '0'))
DSA_STOP = int(os.environ.get('DSA_STOP', '0'))
RW_STOP = int(os.environ.get('RW_STOP', '0'))
ENGS = ("pe", "act", "dve", "pool", "sp")


class Prog:
    def __init__(self, nc, es, n_dma_sems=32):
        self.nc = nc
        self.streams = {e: [] for e in ENGS}
        self.cnt = {e: 0 for e in ENGS}
        self.seen = {e: {} for e in ENGS}
        self.lastw = {}
        self.lastr = {}
        self.n_dma_sems = n_dma_sems
        self.dma_use = [0] * n_dma_sems
        self.dma_next = 0
        self.sems = {}
        for e in ENGS:
            self.sems[e] = es.enter_context(nc.semaphore("s_" + e))
        for i in range(n_dma_sems):
            self.sems[("dma", i)] = es.enter_context(nc.semaphore("s_dma%d" % i))
        self.nins = 0

    def _deps(self, eng, reads, writes):
        deps = {}

        def add(d):
            if d is None:
                return
            k, v = d
            if deps.get(k, 0) < v:
                deps[k] = v

        for k in reads:
            add(self.lastw.get(k))
        for k in writes:
            add(self.lastw.get(k))
            for rk, rv in self.lastr.get(k, {}).items():
                add((rk, rv))
        out = []
        for k, v in deps.items():
            if k == eng and eng == "pe":
                continue
            if self.seen[eng].get(k, 0) >= v:
                continue
            self.seen[eng][k] = v
            out.append((k, v))
        return out

    def _record(self, reads, writes, tok):
        for k in reads:
            d = self.lastr.setdefault(k, {})
            if d.get(tok[0], 0) < tok[1]:
                d[tok[0]] = tok[1]
        for k in writes:
            self.lastw[k] = tok
            self.lastr[k] = {}

    def op(self, eng, fn, reads=(), writes=()):
        waits = self._deps(eng, reads, writes)
        self.cnt[eng] += 1
        tok = (eng, self.cnt[eng])
        self.streams[eng].append((waits, fn, tok))
        self._record(reads, writes, tok)
        self.nins += 1

    def dma(self, q, fn, reads=(), writes=()):
        i = self.dma_next
        self.dma_next = (self.dma_next + 1) % self.n_dma_sems
        waits = self._deps(q, reads, writes)
        prev = self.dma_use[i] * 16
        key = ("dma", i)
        if prev and self.seen[q].get(key, 0) < prev:
            self.seen[q][key] = prev
            waits.append((key, prev))
        self.dma_use[i] += 1
        tok = (key, self.dma_use[i] * 16)
        self.streams[q].append((waits, fn, tok))
        self._record(reads, writes, tok)
        self.nins += 1

    def wait_all(self, eng, keys):
        waits = self._deps(eng, keys, ())
        self.streams[eng].append((waits, None, None))

    def emit(self):
        nc = self.nc
        sems = self.sems
        streams = self.streams
        with nc.Block() as block:
            def run(engobj, name):
                for waits, fn, tok in streams[name]:
                    for k, v in waits:
                        engobj.wait_ge(sems[k], v)
                    if fn is None:
                        continue
                    ins = fn(engobj)
                    ins.then_inc(sems[tok[0]], 16 if isinstance(tok[0], tuple) else 1)

            @block.tensor
            def _(e):
                run(e, "pe")

            @block.scalar
            def _(e):
                run(e, "act")

            @block.vector
            def _(e):
                run(e, "dve")

            @block.gpsimd
            def _(e):
                run(e, "pool")

            @block.sync
            def _(e):
                run(e, "sp")
        self.streams = {e: [] for e in ENGS}


class TT:
    def __init__(self, ap, name):
        self.ap = ap
        self.name = name
        self.shape = tuple(ap.shape)

    def __getitem__(self, idx):
        return self.ap[idx]


ARENA_WORDS = 51200


def build_layer(T, last, dbg=False, branches=(1, 1, 1, 1)):
    TO = T // 2
    NB = T // 512
    NOB = TO // 512
    nc = bass.Bass("TRN2", target_bir_lowering=False)
    dr = {}

    def din(name, shape, dt=F32):
        dr[name] = nc.dram_tensor(name, list(shape), dt, kind="ExternalInput").ap()

    def dout(name, shape, dt=F32):
        dr[name] = nc.dram_tensor(name, list(shape), dt, kind="ExternalOutput").ap()

    din("xT", [D, T]); din("xoT", [D, TO]); din("flags", [128, 2]); din("cT", [128, 8])
    din("pos", [1, T], I32)
    din("ada_w", [D, 6 * D]); din("ada_bT", [128, 48]); din("g1T", [128, 8]); din("g2T", [128, 8]); din("gfT", [128, 8])
    din("w_in", [D, D_IN]); din("gate_w", [D, 4 * D]); din("gate_bT", [128, 32]); din("branch_w", [4, W, D]); din("out_w", [D, D])
    din("rg_conv_wT", [128, 2, 4]); din("rg_conv_bT", [128, 2]); din("rg_wr", [4, 64, 64]); din("rg_wi", [4, 64, 64])
    din("rg_brT", [128, 2]); din("rg_biT", [128, 2]); din("rg_lamT", [128, 2])
    din("router_w", [D, NE]); din("router_b", [1, NE])
    din("rw_upT", [128, 3, 256]); din("rw_prmT", [128, 7, 2]); din("rw_muT", [128, 7]); din("rw_cst", [128, 6, 128]); din("rw_hmask", [128, 2])
    din("rope_c", [128, 4]); din("perm64", [128, 128]); din("perm32", [128, 128]); din("w_ik4", [D, 128])
    din("s5_lreT", [128, 8]); din("s5_limT", [128, 8]); din("s5_lstT", [128, 8]); din("s5_dT", [128, 2]); din("s5_glbT", [128, 2])
    din("s5_brT", [8, 128, 128]); din("s5_biT", [8, 128, 128]); din("s5_crT", [8, 128, 128]); din("s5_ciT", [8, 128, 128]); din("s5_glu_w", [W, W])
    din("exp_w1", [NE, D, DE]); din("exp_w3", [NE, D, DE]); din("exp_w2", [NE, DE, D])
    dout("outT", [D, TO])
    if dbg:
        dout("dbg_br", [4, W, TO]); dout("dbg_xmid", [D, TO])

    top = ExitStack()
    P = Prog(nc, top)
    uid = [0]

    def sb(shape, dt=F32, name="t"):
        uid[0] += 1
        nm = "%s_%d" % (name, uid[0])
        return TT(top.enter_context(nc.sbuf_tensor(nm, list(shape), dt))[:], nm)

    arena = top.enter_context(nc.sbuf_tensor("arena", [128, ARENA_WORDS], F32))
    banks = [TT(top.enter_context(nc.psum_tensor("bank%d" % i, [128, 512], F32))[:], "bank%d" % i) for i in range(7)]
    bank_bf = TT(top.enter_context(nc.psum_tensor("bank7", [128, 1024], BF16))[:], "bank7")

    class Bump:
        def __init__(self, off, end):
            self.off = off
            self.end = end

        def __call__(self, shape, dt=F32, name="a"):
            uid[0] += 1
            nm = "%s_%d" % (name, uid[0])
            n = 1
            for s_ in shape[1:]:
                n *= s_
            esz = 4 if dt in (F32, I32) else 2
            nw = (n * esz + 3) // 4
            w0 = self.off // 4
            assert self.off % 4 == 0 and self.off + nw * 4 <= self.end, (nm, self.off, nw * 4, self.end)
            self.off += nw * 4
            v = arena[0:shape[0], w0:w0 + nw]
            if dt != F32:
                v = v.bitcast(dt)
            v = v[:, 0:n]
            if len(shape) == 3:
                v = v.rearrange("p (a b) -> p a b", b=shape[2])
            elif len(shape) == 4:
                v = v.rearrange("p (a b c) -> p a b c", b=shape[2], c=shape[3])
            return TT(v, nm)

    KB = 1024
    R0, R1, R2, REND = 0, 64 * KB, 96 * KB, 200 * KB
    if T < 4096:
        pass

    ident_b = sb([128, 128], BF16, "identb")
    ident_f = sb([128, 128], F32, "identf")
    ones_f = sb([128, 128], F32, "onesf")
    flags = sb([128, 2], F32, "flags")
    mods = sb([128, 6, 8], F32, "mods")
    A1 = sb([128, 8], F32, "A1")
    A2 = sb([128, 8], F32, "A2")
    gf = sb([128, 8], F32, "gf")
    zero8 = sb([128, 8], F32, "zero8")
    epsb = sb([128, 1], F32, "epsb")
    hT = Bump(R0, R1)([128, 8, T], BF16, "hT")
    br_own = Bump(R1, R2)([128, 8, TO], BF16, "br_own")
    MODK = [mods.name, A1.name, A2.name, gf.name]

    for t_ in (ident_b, ident_f):
        P.op("pool", lambda e, t_=t_: e.memset(t_[:], 1.0), writes=[t_.name])
        P.op("pool", lambda e, t_=t_: e.affine_select(out=t_[:], in_=t_[:], pattern=[[-1, 128]], compare_op=ALU.is_equal,
                                                      fill=0.0, base=0, channel_multiplier=1), reads=[t_.name], writes=[t_.name])
    P.op("pool", lambda e: e.memset(ones_f[:], 1.0), writes=[ones_f.name])
    P.op("pool", lambda e: e.memset(zero8[:], 0.0), writes=[zero8.name])
    P.op("pool", lambda e: e.memset(epsb[:], EPS), writes=[epsb.name])
    P.op("pool", lambda e: e.memset(br_own[:], 0.0), writes=[br_own.name])
    P.dma("sp", lambda e: e.dma_start(out=flags[:], in_=dr["flags"]), writes=[flags.name])
    P.dma("sp", lambda e: e.dma_start(out=gf[:], in_=dr["gfT"]), writes=[gf.name])

    al = Bump(R2, REND)
    cact = al([128, 8]); abT = al([128, 48]); g1 = al([128, 8]); g2 = al([128, 8])
    wst = [al([128, 8, 512], F32, "adaw") for _ in range(2)]
    pm = banks[0]
    for t_, n_ in ((cact, "cT"), (abT, "ada_bT"), (g1, "g1T"), (g2, "g2T")):
        P.dma("sp", lambda e, t_=t_, n_=n_: e.dma_start(out=t_[:], in_=dr[n_]), writes=[t_.name])
    P.op("act", lambda e: e.activation(out=cact[:], in_=cact[:], func=AF.Silu), reads=[cact.name], writes=[cact.name])
    awv = dr["ada_w"].rearrange("(k p) n -> p k n", p=128)
    for h in range(12):
        wt = wst[h % 2]
        P.dma("sp" if h % 2 == 0 else "pool", lambda e, wt=wt, h=h: e.dma_start(out=wt[:], in_=awv[:, :, h * 512:(h + 1) * 512]), writes=[wt.name])
        for c4 in range(4):
            col = h * 4 + c4
            for k in range(8):
                P.op("pe", lambda e, wt=wt, c4=c4, col=col, k=k: e.matmul(pm[:, col:col + 1], lhsT=wt[:, k, c4 * 128:(c4 + 1) * 128],
                                                                          rhs=cact[:, k:k + 1], start=(k == 0), stop=(k == 7)),
                     reads=[wt.name, cact.name], writes=[pm.name])
    modsf = mods[:].rearrange("p a b -> p (a b)")
    P.op("dve", lambda e: e.tensor_tensor(out=modsf, in0=pm[:, 0:48], in1=abT[:], op=ALU.add), reads=[pm.name, abT.name], writes=[mods.name])
    P.op("dve", lambda e: e.scalar_tensor_tensor(out=A1[:], in0=mods[:, 1, :], scalar=1.0, in1=g1[:], op0=ALU.add, op1=ALU.mult),
         reads=[mods.name, g1.name], writes=[A1.name])
    P.op("dve", lambda e: e.scalar_tensor_tensor(out=A2[:], in0=mods[:, 4, :], scalar=1.0, in1=g2[:], op0=ALU.add, op1=ALU.mult),
         reads=[mods.name, g2.name], writes=[A2.name])
    P.emit()

    def norm_block(src, sk, Aap, Bap, sq, pss, rstd, out_bf=None, okb=None, out_f=None, okf=None):
        P.op("act", lambda e: e.activation(out=sq[:], in_=src, func=AF.Square), reads=[sk], writes=[sq.name])
        for k in range(8):
            P.op("pe", lambda e, k=k: e.matmul(pss[:], lhsT=ones_f[:], rhs=sq[:, k, :], start=(k == 0), stop=(k == 7)),
                 reads=[sq.name, ones_f.name], writes=[pss.name])
        P.op("act", lambda e: e.activation(out=rstd[:], in_=pss[:], func=AF.Sqrt, scale=1.0 / D, bias=epsb[:]), reads=[pss.name, epsb.name], writes=[rstd.name])
        P.op("dve", lambda e: e.reciprocal(out=rstd[:], in_=rstd[:]), reads=[rstd.name], writes=[rstd.name])
        for k in range(8):
            P.op("dve", lambda e, k=k: e.tensor_tensor(out=sq[:, k, :], in0=src[:, k, :], in1=rstd[:], op=ALU.mult),
                 reads=[sk, rstd.name, sq.name], writes=[sq.name])
            if out_bf is not None:
                P.op("act", lambda e, k=k: e.activation(out=out_bf[:, k, :], in_=sq[:, k, :], func=AF.Identity, scale=Aap[:, k:k + 1], bias=Bap[:, k:k + 1]),
                     reads=[sq.name] + MODK, writes=[okb])
            if out_f is not None:
                P.op("pool", lambda e, k=k: e.tensor_scalar(out=out_f[:, k, :], in0=sq[:, k, :], scalar1=Aap[:, k:k + 1], scalar2=Bap[:, k:k + 1], op0=ALU.mult, op1=ALU.add),
                     reads=[sq.name] + MODK, writes=[okf])

    def sel_own(eng, dst, src, tmp, rk, wk_, tk):
        sv = src.rearrange("p (a two c) -> p a two c", two=2, c=128)
        dv = dst.rearrange("p (a c) -> p a c", c=128)
        tv = tmp.rearrange("p (a c) -> p a c", c=128)
        P.op(eng, lambda e: e.tensor_scalar(out=tv, in0=sv[:, :, 0, :], scalar1=flags[:, 0:1], scalar2=None, op0=ALU.mult), reads=[rk, flags.name], writes=[tk])
        P.op("dve", lambda e: e.scalar_tensor_tensor(out=dv, in0=sv[:, :, 1, :], scalar=flags[:, 1:2], in1=tv, op0=ALU.mult, op1=ALU.add),
             reads=[rk, tk, flags.name], writes=[wk_])

    def load_w_bf16(dst, src_view, ncols, stg, key, q="sp", ceng="pool"):
        step = stg[0].shape[2]
        for i, c0 in enumerate(range(0, ncols, step)):
            c1 = min(ncols, c0 + step)
            st = stg[i % len(stg)]
            P.dma(q, lambda e, st=st, c0=c0, c1=c1: e.dma_start(out=st[:, :, 0:c1 - c0], in_=src_view[:, :, c0:c1]), writes=[st.name])
            P.op(ceng, lambda e, st=st, c0=c0, c1=c1: e.tensor_copy(out=dst[:, :, c0:c1], in_=st[:, :, 0:c1 - c0]), reads=[st.name], writes=[key])

    winv = dr["w_in"].rearrange("(k p) n -> p k n", p=128)

    al = Bump(R2, REND)
    xs = [al([128, 8, 512], F32, "xs") for _ in range(2)]
    sq = al([128, 8, 512], F32, "sq"); rstd = al([128, 512], F32, "rstd")
    xv = dr["xT"].rearrange("(k p) t -> p k t", p=128)
    for nb in range(NB):
        x_ = xs[nb % 2]
        P.dma("sp", lambda e, x_=x_, nb=nb: e.dma_start(out=x_[:], in_=xv[:, :, nb * 512:(nb + 1) * 512]), writes=[x_.name])
        norm_block(x_[:], x_.name, A1, mods[:, 0, :], sq, banks[nb % 2], rstd, out_bf=hT[:, :, nb * 512:(nb + 1) * 512], okb=hT.name)
    P.emit()

    if branches[2]:
        al = Bump(R2, REND)
        stg = [al([128, 8, 128], F32, "stg") for _ in range(2)]
        wrg = al([128, 8, 512], BF16, "wrg")
        load_w_bf16(wrg, winv[:, :, C_RGX:C_RGX + 512], 512, stg, wrg.name)
        cw = al([128, 2, 4]); cb = al([128, 2]); brT = al([128, 2]); biT = al([128, 2]); lam = al([128, 2]); sc8 = al([128, 2], F32, "sc8")
        for t_, n_ in ((cw, "rg_conv_wT"), (cb, "rg_conv_bT"), (brT, "rg_brT"), (biT, "rg_biT"), (lam, "rg_lamT")):
            P.dma("sp", lambda e, t_=t_, n_=n_: e.dma_start(out=t_[:], in_=dr[n_]), writes=[t_.name])
        P.op("act", lambda e: e.activation(out=sc8[:], in_=lam[:], func=AF.Exp, scale=-1.0), reads=[lam.name], writes=[sc8.name])
        P.op("act", lambda e: e.activation(out=sc8[:], in_=sc8[:], func=AF.Ln, bias=1.0), reads=[sc8.name], writes=[sc8.name])
        P.op("dve", lambda e: e.tensor_scalar(out=sc8[:], in0=sc8[:], scalar1=-8.0, scalar2=None, op0=ALU.mult), reads=[sc8.name], writes=[sc8.name])
        wbd_f = al([128, 2, 2, 128], F32, "wbdf")
        wbd = al([128, 2, 2, 128], BF16, "wbd")
        P.op("pool", lambda e: e.memset(wbd_f[:], 0.0), writes=[wbd_f.name])
        for ct in range(2):
            for ri, nm in enumerate(("rg_wr", "rg_wi")):
                for hb in range(2):
                    P.dma("sp", lambda e, ct=ct, ri=ri, nm=nm, hb=hb: e.dma_start(out=wbd_f[hb * 64:(hb + 1) * 64, ct, ri, hb * 64:(hb + 1) * 64],
                                                                                 in_=dr[nm][2 * ct + hb]), writes=[wbd_f.name])
        P.op("pool", lambda e: e.tensor_copy(out=wbd[:], in_=wbd_f[:]), reads=[wbd_f.name], writes=[wbd.name])
        bX = al([128, T + 4], F32, "bX"); bC = al([128, T], F32, "bC"); bR = al([128, T], F32, "bR")
        bI = al([128, T], F32, "bI"); bG = al([128, T], BF16, "bG"); bcb = al([128, T], BF16, "bcb")
        pp = banks[0:4]
        for ct in range(2):
            P.op("pool", lambda e: e.memset(bX[:, 0:4], 0.0), writes=[bX.name])
            for nb in range(NB):
                sl = slice(nb * 512, (nb + 1) * 512)
                pa, pb = pp[(2 * nb) % 4], pp[(2 * nb + 1) % 4]
                for k in range(8):
                    P.op("pe", lambda e, k=k, pa=pa, sl=sl, ct=ct: e.matmul(pa[:], lhsT=wrg[:, k, ct * 128:(ct + 1) * 128], rhs=hT[:, k, sl], start=(k == 0), stop=(k == 7)),
                         reads=[wrg.name, hT.name], writes=[pa.name])
                for k in range(8):
                    P.op("pe", lambda e, k=k, pb=pb, sl=sl, ct=ct: e.matmul(pb[:], lhsT=wrg[:, k, 256 + ct * 128:256 + (ct + 1) * 128], rhs=hT[:, k, sl], start=(k == 0), stop=(k == 7)),
                         reads=[wrg.name, hT.name], writes=[pb.name])
                P.op("dve", lambda e, pa=pa, nb=nb: e.tensor_copy(out=bX[:, 4 + nb * 512:4 + (nb + 1) * 512], in_=pa[:]), reads=[pa.name], writes=[bX.name])
                P.op("act", lambda e, pb=pb, sl=sl: e.activation(out=bG[:, sl], in_=pb[:], func=AF.Gelu), reads=[pb.name], writes=[bG.name])
            P.op("dve", lambda e, ct=ct: e.tensor_scalar(out=bC[:], in0=bX[:, 4:4 + T], scalar1=cw[:, ct, 3:4], scalar2=cb[:, ct:ct + 1], op0=ALU.mult, op1=ALU.add),
                 reads=[bX.name, cw.name, cb.name], writes=[bC.name])
            for kk in range(3):
                P.op("dve", lambda e, ct=ct, kk=kk: e.scalar_tensor_tensor(out=bC[:], in0=bX[:, 1 + kk:1 + kk + T], scalar=cw[:, ct, kk:kk + 1], in1=bC[:], op0=ALU.mult, op1=ALU.add),
                     reads=[bX.name, bC.name, cw.name], writes=[bC.name])
            P.op("pool", lambda e: e.tensor_copy(out=bcb[:], in_=bC[:]), reads=[bC.name], writes=[bcb.name])
            for nb in range(NB):
                sl = slice(nb * 512, (nb + 1) * 512)
                pa, pb = pp[(2 * nb) % 4], pp[(2 * nb + 1) % 4]
                P.op("pe", lambda e, pa=pa, sl=sl, ct=ct: e.matmul(pa[:], lhsT=wbd[:, ct, 0, :], rhs=bcb[:, sl], start=True, stop=True), reads=[wbd.name, bcb.name], writes=[pa.name])
                P.op("pe", lambda e, pb=pb, sl=sl, ct=ct: e.matmul(pb[:], lhsT=wbd[:, ct, 1, :], rhs=bcb[:, sl], start=True, stop=True), reads=[wbd.name, bcb.name], writes=[pb.name])
                P.op("act", lambda e, pa=pa, sl=sl, ct=ct: e.activation(out=bR[:, sl], in_=pa[:], func=AF.Sigmoid, bias=brT[:, ct:ct + 1]), reads=[pa.name, brT.name], writes=[bR.name])
                P.op("act", lambda e, pb=pb, sl=sl, ct=ct: e.activation(out=bI[:, sl], in_=pb[:], func=AF.Sigmoid, bias=biT[:, ct:ct + 1]), reads=[pb.name, biT.name], writes=[bI.name])
            P.op("act", lambda e, ct=ct: e.activation(out=bR[:], in_=bR[:], func=AF.Exp, scale=sc8[:, ct:ct + 1]), reads=[bR.name, sc8.name], writes=[bR.name])
            P.op("dve", lambda e: e.tensor_tensor(out=bI[:], in0=bI[:], in1=bC[:], op=ALU.mult), reads=[bI.name, bC.name], writes=[bI.name])
            P.op("pool", lambda e: e.tensor_tensor(out=bC[:], in0=bR[:], in1=bR[:], op=ALU.mult), reads=[bR.name, bC.name, bI.name], writes=[bC.name])
            P.op("act", lambda e: e.activation(out=bC[:], in_=bC[:], func=AF.Sqrt, scale=-1.0, bias=1.0), reads=[bC.name], writes=[bC.name])
            P.op("dve", lambda e: e.tensor_tensor(out=bI[:], in0=bI[:], in1=bC[:], op=ALU.mult), reads=[bI.name, bC.name], writes=[bI.name])
            P.op("dve", lambda e: e.tensor_tensor_scan(out=bC[:], data0=bR[:], data1=bI[:], initial=0.0, op0=ALU.mult, op1=ALU.add), reads=[bR.name, bI.name, bC.name], writes=[bC.name])
            P.op("dve", lambda e: e.tensor_tensor(out=bC[:], in0=bC[:], in1=bG[:], op=ALU.mult), reads=[bC.name, bG.name], writes=[bC.name])
            sel_own("pool", br_own[:, 4 + ct, :], bC[:], bI[:, 0:TO], bC.name, br_own.name, bI.name)
        P.emit()

    TWO_PI = 6.283185

    def sin_2pi(y, out, wi, wf, wm, key_in, n):
        P.op("dve", lambda e: e.tensor_copy(out=wi, in_=y), reads=[key_in], writes=["sc_wi"])
        P.op("dve", lambda e: e.tensor_copy(out=wf, in_=wi), reads=["sc_wi"], writes=["sc_wf"])
        P.op("dve", lambda e: e.tensor_tensor(out=wf, in0=y, in1=wf, op=ALU.subtract), reads=[key_in, "sc_wf"], writes=["sc_wf"])
        P.op("dve", lambda e: e.tensor_scalar(out=wm, in0=wf, scalar1=0.5, scalar2=None, op0=ALU.is_gt), reads=["sc_wf"], writes=["sc_wm"])
        P.op("dve", lambda e: e.tensor_tensor(out=wf, in0=wf, in1=wm, op=ALU.subtract), reads=["sc_wf", "sc_wm"], writes=["sc_wf"])
        P.op("dve", lambda e: e.tensor_scalar(out=wm, in0=wf, scalar1=-0.5, scalar2=None, op0=ALU.is_lt), reads=["sc_wf"], writes=["sc_wm"])
        P.op("dve", lambda e: e.tensor_tensor(out=wf, in0=wf, in1=wm, op=ALU.add), reads=["sc_wf", "sc_wm"], writes=["sc_wf"])
        P.op("act", lambda e: e.activation(out=out, in_=wf, func=AF.Sin, scale=TWO_PI), reads=["sc_wf"], writes=["sc_out"])

    if branches[0]:
        al = Bump(R2, REND)
        stg = [al([128, 8, 128], F32, "stg") for _ in range(2)]
        wu = al([128, 8, 256], BF16, "wu")
        load_w_bf16(wu, winv[:, :, C_U:C_U + 256], 256, stg, wu.name)
        lre = al([128, 8]); lim = al([128, 8]); lst = al([128, 8]); dsk = al([128, 2]); glb = al([128, 2])
        for t_, n_ in ((lre, "s5_lreT"), (lim, "s5_limT"), (lst, "s5_lstT"), (dsk, "s5_dT"), (glb, "s5_glbT")):
            P.dma("sp", lambda e, t_=t_, n_=n_: e.dma_start(out=t_[:], in_=dr[n_]), writes=[t_.name])
        sm = {n_: al([128, 8], F32, "s5_" + n_) for n_ in ("th", "thn", "rho", "c1", "s1", "c512", "s512", "nr", "ni", "den", "kr", "ki", "t0", "t1", "y")}
        smi = al([128, 8], I32, "s5_i")
        SK = "s5small"

        def sop(eng, fn):
            P.op(eng, fn, reads=[SK, lre.name, lim.name, lst.name], writes=[SK])

        INV2PI = 1.0 / (2.0 * math.pi)
        smk = al([128, 8], I32, "s5_k"); sme = al([128, 8], I32, "s5_e"); smr = al([128, 8], F32, "s5_r"); smp = al([128, 8], F32, "s5_p")

        def exp_poly(out, x):
            sop("dve", lambda e: e.tensor_scalar(out=smp[:], in0=x, scalar1=1.4426950408889634, scalar2=None, op0=ALU.mult))
            sop("dve", lambda e: e.tensor_copy(out=smk[:], in_=smp[:]))
            sop("dve", lambda e: e.tensor_copy(out=smp[:], in_=smk[:]))
            sop("dve", lambda e: e.scalar_tensor_tensor(out=smr[:], in0=smp[:], scalar=-0.693359375, in1=x, op0=ALU.mult, op1=ALU.add))
            sop("dve", lambda e: e.scalar_tensor_tensor(out=smr[:], in0=smp[:], scalar=2.12194440e-4, in1=smr[:], op0=ALU.mult, op1=ALU.add))
            cs = [1.0 / math.factorial(i_) for i_ in range(8)]
            sop("dve", lambda e: e.tensor_scalar(out=smp[:], in0=smr[:], scalar1=cs[7], scalar2=cs[6], op0=ALU.mult, op1=ALU.add))
            for i_ in range(5, -1, -1):
                sop("dve", lambda e: e.tensor_tensor(out=smp[:], in0=smp[:], in1=smr[:], op=ALU.mult))
                sop("dve", lambda e, i_=i_: e.tensor_scalar(out=smp[:], in0=smp[:], scalar1=cs[i_], scalar2=None, op0=ALU.add))
            sop("dve", lambda e: e.tensor_scalar(out=sme[:], in0=smk[:], scalar1=127.0, scalar2=8388608.0, op0=ALU.add, op1=ALU.mult))
            sop("dve", lambda e: e.tensor_tensor(out=out, in0=smp[:], in1=sme[:].bitcast(F32), op=ALU.mult))

        exp_poly(sm["t0"][:], lst[:])
        sop("dve", lambda e: e.tensor_tensor(out=sm["th"][:], in0=lim[:], in1=sm["t0"][:], op=ALU.mult))
        sop("dve", lambda e: e.tensor_tensor(out=sm["t1"][:], in0=lre[:], in1=sm["t0"][:], op=ALU.mult))
        exp_poly(sm["rho"][:], sm["t1"][:])
        sop("dve", lambda e: e.tensor_scalar(out=sm["thn"][:], in0=sm["th"][:], scalar1=INV2PI, scalar2=None, op0=ALU.mult))

        def small_sincos(scale_mul, out_s, out_c):
            sop("dve", lambda e: e.tensor_scalar(out=sm["y"][:], in0=sm["thn"][:], scalar1=float(scale_mul), scalar2=None, op0=ALU.mult))
            sin_2pi(sm["y"][:], out_s[:], smi[:], sm["t0"][:], sm["t1"][:], SK, 8)
            P.op("dve", lambda e: e.tensor_scalar(out=sm["y"][:], in0=sm["y"][:], scalar1=0.25, scalar2=None, op0=ALU.add), reads=[SK, "sc_out"], writes=[SK])
            sin_2pi(sm["y"][:], out_c[:], smi[:], sm["t0"][:], sm["t1"][:], SK, 8)
            P.op("dve", lambda e: e.tensor_copy(out=sm["y"][:], in_=sm["y"][:]), reads=[SK, "sc_out", "sc_wf", "sc_wm", "sc_wi"], writes=[SK, "sc_out", "sc_wf", "sc_wm", "sc_wi"])

        small_sincos(1.0, sm["s1"], sm["c1"])
        small_sincos(512.0, sm["s512"], sm["c512"])
        sop("dve", lambda e: e.tensor_tensor(out=sm["nr"][:], in0=sm["rho"][:], in1=sm["c1"][:], op=ALU.mult))
        sop("dve", lambda e: e.tensor_scalar(out=sm["nr"][:], in0=sm["nr"][:], scalar1=-1.0, scalar2=None, op0=ALU.add))
        sop("dve", lambda e: e.tensor_tensor(out=sm["ni"][:], in0=sm["rho"][:], in1=sm["s1"][:], op=ALU.mult))
        sop("dve", lambda e: e.tensor_tensor(out=sm["den"][:], in0=lre[:], in1=lre[:], op=ALU.mult))
        sop("dve", lambda e: e.tensor_tensor(out=sm["t0"][:], in0=lim[:], in1=lim[:], op=ALU.mult))
        sop("dve", lambda e: e.tensor_tensor(out=sm["den"][:], in0=sm["den"][:], in1=sm["t0"][:], op=ALU.add))
        sop("dve", lambda e: e.reciprocal(out=sm["den"][:], in_=sm["den"][:]))
        sop("dve", lambda e: e.tensor_tensor(out=sm["kr"][:], in0=sm["nr"][:], in1=lre[:], op=ALU.mult))
        sop("dve", lambda e: e.tensor_tensor(out=sm["t0"][:], in0=sm["ni"][:], in1=lim[:], op=ALU.mult))
        sop("dve", lambda e: e.tensor_tensor(out=sm["kr"][:], in0=sm["kr"][:], in1=sm["t0"][:], op=ALU.add))
        sop("dve", lambda e: e.tensor_tensor(out=sm["kr"][:], in0=sm["kr"][:], in1=sm["den"][:], op=ALU.mult))
        sop("dve", lambda e: e.tensor_tensor(out=sm["ki"][:], in0=sm["ni"][:], in1=lre[:], op=ALU.mult))
        sop("dve", lambda e: e.tensor_tensor(out=sm["t0"][:], in0=sm["nr"][:], in1=lim[:], op=ALU.mult))
        sop("dve", lambda e: e.tensor_tensor(out=sm["ki"][:], in0=sm["ki"][:], in1=sm["t0"][:], op=ALU.subtract))
        sop("dve", lambda e: e.tensor_tensor(out=sm["ki"][:], in0=sm["ki"][:], in1=sm["den"][:], op=ALU.mult))

        ubf = al([128, 2, T], BF16, "ubf")
        yacc = al([128, 2, T], F32, "yacc")
        iot = al([128, 512], F32, "iot")
        P.op("pool", lambda e: e.iota(iot[:], pattern=[[1, 512]], base=0, channel_multiplier=0, allow_small_or_imprecise_dtypes=True), writes=[iot.name])
        for ut in range(2):
            for nb in range(NB):
                sl = slice(nb * 512, (nb + 1) * 512)
                pa = banks[(ut * NB + nb) % 4]
                for k in range(8):
                    P.op("pe", lambda e, k=k, pa=pa, sl=sl, ut=ut: e.matmul(pa[:], lhsT=wu[:, k, ut * 128:(ut + 1) * 128], rhs=hT[:, k, sl], start=(k == 0), stop=(k == 7)),
                         reads=[wu.name, hT.name], writes=[pa.name])
                P.op("act", lambda e, pa=pa, sl=sl, ut=ut: e.copy(out=ubf[:, ut, sl], in_=pa[:]), reads=[pa.name], writes=[ubf.name])
        tb_off = al.off
        tb = {n_: al([128, 512], F32, "s5t_" + n_) for n_ in ("sin", "cos", "ck", "sk", "nsin", "t1", "t2", "t3", "t4", "br", "bi", "gr", "gi", "o1", "o2")}
        tb["y"], tb["wf"], tb["wm"] = tb["t2"], tb["t3"], tb["t4"]
        twi = al([128, 512], I32, "s5t_wi")
        hr = al([128, 512], F32, "hr"); hni = al([128, 512], F32, "hni")
        ini = al([128, 4], F32, "ini")
        rho_t = al([128, 512], F32, "rho_t")
        bw_f = al([128, 4, 128], F32, "bwf"); bw = al([128, 4, 128], BF16, "bw")
        TK = "s5tab"
        for s in range(8):
            ut = s // 4
            for i_, nm in enumerate(("s5_brT", "s5_biT", "s5_crT", "s5_ciT")):
                P.dma("sp", lambda e, i_=i_, nm=nm, s=s: e.dma_start(out=bw_f[:, i_, :], in_=dr[nm][s]), writes=[bw_f.name])
            P.op("pool", lambda e: e.tensor_copy(out=bw[:], in_=bw_f[:]), reads=[bw_f.name], writes=[bw.name])
            P.op("dve", lambda e, s=s: e.tensor_scalar(out=tb["y"][:], in0=iot[:], scalar1=sm["thn"][:, s:s + 1], scalar2=None, op0=ALU.mult), reads=[iot.name, SK, TK, "s5t2", "s5t3", "s5t4"], writes=[TK, "s5gate", "s5t2", "s5t3", "s5t4", "sc_wf", "sc_wm", "sc_out"])
            sin_2pi(tb["y"][:], tb["sin"][:], twi[:], tb["wf"][:], tb["wm"][:], TK, 512)
            P.op("dve", lambda e: e.tensor_scalar(out=tb["y"][:], in0=tb["y"][:], scalar1=0.25, scalar2=None, op0=ALU.add), reads=[TK, "sc_out"], writes=[TK])
            sin_2pi(tb["y"][:], tb["cos"][:], twi[:], tb["wf"][:], tb["wm"][:], TK, 512)
            P.op("dve", lambda e, s=s: e.tensor_scalar(out=tb["t1"][:], in0=tb["sin"][:], scalar1=sm["ki"][:, s:s + 1], scalar2=None, op0=ALU.mult), reads=[TK, "sc_out", SK], writes=[TK])
            P.op("dve", lambda e, s=s: e.scalar_tensor_tensor(out=tb["ck"][:], in0=tb["cos"][:], scalar=sm["kr"][:, s:s + 1], in1=tb["t1"][:], op0=ALU.mult, op1=ALU.add), reads=[TK, SK], writes=[TK])
            P.op("dve", lambda e, s=s: e.tensor_scalar(out=tb["t1"][:], in0=tb["sin"][:], scalar1=sm["kr"][:, s:s + 1], scalar2=None, op0=ALU.mult), reads=[TK, SK], writes=[TK])
            P.op("dve", lambda e, s=s: e.scalar_tensor_tensor(out=tb["sk"][:], in0=tb["cos"][:], scalar=sm["ki"][:, s:s + 1], in1=tb["t1"][:], op0=ALU.mult, op1=ALU.subtract), reads=[TK, SK], writes=[TK])
            P.op("dve", lambda e: e.tensor_scalar(out=tb["nsin"][:], in0=tb["sin"][:], scalar1=-1.0, scalar2=None, op0=ALU.mult), reads=[TK], writes=[TK, "s5gate"])
            P.op("dve", lambda e, s=s: e.tensor_copy(out=rho_t[:], in_=sm["rho"][:, s:s + 1].to_broadcast([128, 512])), reads=[SK, "s5gr", "s5gi", "s5rho"], writes=["s5rho"])
            rho_b = rho_t[:]
            for nb in range(NB):
                sl = slice(nb * 512, (nb + 1) * 512)
                pr, pi_, py = banks[4 + (nb % 2)], banks[2 + (nb % 2)], banks[(nb % 2)]
                P.op("pe", lambda e, pr=pr, sl=sl, ut=ut: e.matmul(pr[:], lhsT=bw[:, 0, :], rhs=ubf[:, ut, sl], start=True, stop=True), reads=[bw.name, ubf.name], writes=[pr.name])
                P.op("pe", lambda e, pi_=pi_, sl=sl, ut=ut: e.matmul(pi_[:], lhsT=bw[:, 1, :], rhs=ubf[:, ut, sl], start=True, stop=True), reads=[bw.name, ubf.name], writes=[pi_.name])
                P.op("dve", lambda e, pr=pr: e.tensor_tensor(out=tb["t1"][:], in0=pr[:], in1=tb["ck"][:], op=ALU.mult), reads=[pr.name, "s5gate"], writes=["s5t1"])
                P.op("dve", lambda e, pi_=pi_: e.tensor_tensor(out=tb["t2"][:], in0=pi_[:], in1=tb["sk"][:], op=ALU.mult), reads=[pi_.name, "s5gate"], writes=["s5t2"])
                P.op("dve", lambda e, pi_=pi_: e.tensor_tensor(out=tb["t3"][:], in0=pi_[:], in1=tb["ck"][:], op=ALU.mult), reads=[pi_.name, "s5gate"], writes=["s5t3"])
                P.op("dve", lambda e, pr=pr: e.tensor_tensor(out=tb["t4"][:], in0=pr[:], in1=tb["sk"][:], op=ALU.mult), reads=[pr.name, "s5gate"], writes=["s5t4"])
                P.op("pool", lambda e: e.tensor_tensor(out=tb["br"][:], in0=tb["t1"][:], in1=tb["t2"][:], op=ALU.subtract), reads=["s5t1", "s5t2"], writes=["s5br"])
                P.op("pool", lambda e: e.tensor_tensor(out=tb["bi"][:], in0=tb["t3"][:], in1=tb["t4"][:], op=ALU.add), reads=["s5t3", "s5t4"], writes=["s5bi"])
                if nb == 0:
                    P.op("dve", lambda e: e.memset(ini[:], 0.0), reads=["s5ini"], writes=["s5ini"])
                P.op("dve", lambda e: e.tensor_tensor_scan(out=tb["gr"][:], data0=rho_b, data1=tb["br"][:], initial=ini[:, 0:1], op0=ALU.mult, op1=ALU.add),
                     reads=["s5br", "s5ini", SK, "s5rho"], writes=["s5gr"])
                P.op("dve", lambda e: e.tensor_tensor_scan(out=tb["gi"][:], data0=rho_b, data1=tb["bi"][:], initial=ini[:, 1:2], op0=ALU.mult, op1=ALU.add),
                     reads=["s5bi", "s5ini", SK, "s5rho"], writes=["s5gi"])
                P.op("dve", lambda e, s=s: e.tensor_scalar(out=ini[:, 2:3], in0=tb["gi"][:, 511:512], scalar1=sm["s512"][:, s:s + 1], scalar2=None, op0=ALU.mult), reads=["s5gi", SK], writes=["s5ini2"])
                P.op("dve", lambda e, s=s: e.tensor_scalar(out=ini[:, 3:4], in0=tb["gr"][:, 511:512], scalar1=sm["s512"][:, s:s + 1], scalar2=None, op0=ALU.mult), reads=["s5gr", SK], writes=["s5ini3"])
                P.op("dve", lambda e, s=s: e.scalar_tensor_tensor(out=ini[:, 0:1], in0=tb["gr"][:, 511:512], scalar=sm["c512"][:, s:s + 1], in1=ini[:, 2:3], op0=ALU.mult, op1=ALU.subtract),
                     reads=["s5gr", "s5ini2", SK, "s5ini"], writes=["s5ini"])
                P.op("dve", lambda e, s=s: e.scalar_tensor_tensor(out=ini[:, 1:2], in0=tb["gi"][:, 511:512], scalar=sm["c512"][:, s:s + 1], in1=ini[:, 3:4], op0=ALU.mult, op1=ALU.add),
                     reads=["s5gi", "s5ini3", SK, "s5ini"], writes=["s5ini"])
                P.op("pool", lambda e: e.tensor_tensor(out=tb["o1"][:], in0=tb["gr"][:], in1=tb["cos"][:], op=ALU.mult), reads=["s5gr", "s5gate"], writes=["s5o1"])
                P.op("pool", lambda e: e.tensor_tensor(out=tb["o2"][:], in0=tb["gi"][:], in1=tb["nsin"][:], op=ALU.mult), reads=["s5gi", "s5gate"], writes=["s5o2"])
                P.op("pool", lambda e: e.tensor_tensor(out=hr[:], in0=tb["o1"][:], in1=tb["o2"][:], op=ALU.add), reads=["s5o1", "s5o2"], writes=[hr.name])
                P.op("pool", lambda e: e.tensor_tensor(out=tb["o1"][:], in0=tb["gr"][:], in1=tb["nsin"][:], op=ALU.mult), reads=["s5gr", "s5gate", "s5o1"], writes=["s5o1"])
                P.op("pool", lambda e: e.tensor_tensor(out=tb["o2"][:], in0=tb["gi"][:], in1=tb["cos"][:], op=ALU.mult), reads=["s5gi", "s5gate", "s5o2"], writes=["s5o2"])
                P.op("pool", lambda e: e.tensor_tensor(out=hni[:], in0=tb["o1"][:], in1=tb["o2"][:], op=ALU.subtract), reads=["s5o1", "s5o2"], writes=[hni.name])
                P.op("pe", lambda e, py=py: e.matmul(py[:], lhsT=bw_f[:, 2, :], rhs=hr[:], start=True, stop=False), reads=[bw_f.name, hr.name], writes=[py.name])
                P.op("pe", lambda e, py=py: e.matmul(py[:], lhsT=bw_f[:, 3, :], rhs=hni[:], start=False, stop=True), reads=[bw_f.name, hni.name], writes=[py.name])
                if s % 4 == 0:
                    P.op("act", lambda e, py=py, sl=sl, ut=ut: e.copy(out=yacc[:, ut, sl], in_=py[:]), reads=[py.name], writes=[yacc.name])
                else:
                    P.op("dve", lambda e, py=py, sl=sl, ut=ut: e.tensor_tensor(out=yacc[:, ut, sl], in0=yacc[:, ut, sl], in1=py[:], op=ALU.add), reads=[py.name, yacc.name], writes=[yacc.name])
        P.emit()
        al3 = Bump(tb_off, REND)
        ygb = al3([128, 2, T], BF16, "ygb")
        gst = al3([128, 2, 256], F32, "gst"); gwb = al3([128, 2, 256], BF16, "gwb")
        P.dma("sp", lambda e: e.dma_start(out=gst[:], in_=dr["s5_glu_w"].rearrange("(k p) n -> p k n", p=128)), writes=[gst.name])
        P.op("pool", lambda e: e.tensor_copy(out=gwb[:], in_=gst[:]), reads=[gst.name], writes=[gwb.name])
        for ut in range(2):
            P.op("dve", lambda e, ut=ut: e.scalar_tensor_tensor(out=yacc[:, ut, :], in0=ubf[:, ut, :], scalar=dsk[:, ut:ut + 1], in1=yacc[:, ut, :], op0=ALU.mult, op1=ALU.add),
                 reads=[ubf.name, yacc.name, dsk.name], writes=[yacc.name])
            if S5DBG:
                sel_own("pool", br_own[:, 0 + ut, :], yacc[:, ut, :], ygb[:].rearrange("p a t -> p (a t)").bitcast(F32)[:, 0:TO], yacc.name, br_own.name, ygb.name)
            P.op("act", lambda e, ut=ut: e.activation(out=ygb[:, ut, :], in_=yacc[:, ut, :], func=AF.Gelu), reads=[yacc.name], writes=[ygb.name])
        for ut in range(0 if not S5DBG else 2, 2):
            for nb in range(NB):
                sl = slice(nb * 512, (nb + 1) * 512)
                pa = banks[(ut * NB + nb) % 4]
                for k in range(2):
                    P.op("pe", lambda e, k=k, pa=pa, sl=sl, ut=ut: e.matmul(pa[:], lhsT=gwb[:, k, ut * 128:(ut + 1) * 128], rhs=ygb[:, k, sl], start=(k == 0), stop=(k == 1)),
                         reads=[gwb.name, ygb.name], writes=[pa.name])
                P.op("act", lambda e, pa=pa, sl=sl, ut=ut: e.activation(out=yacc[:, ut, sl], in_=pa[:], func=AF.Sigmoid, bias=glb[:, ut:ut + 1]), reads=[pa.name, glb.name, yacc.name], writes=[yacc.name])
            P.op("dve", lambda e, ut=ut: e.tensor_tensor(out=yacc[:, ut, :], in0=yacc[:, ut, :], in1=ygb[:, ut, :], op=ALU.mult), reads=[yacc.name, ygb.name], writes=[yacc.name])
            sel_own("pool", br_own[:, 0 + ut, :], yacc[:, ut, :], ubf[:].rearrange("p a t -> p (a t)").bitcast(F32)[:, 0:TO], yacc.name, br_own.name, ubf.name)
        P.emit()

    if branches[3]:
        LC = 64
        al = Bump(R2, REND)
        stg = [al([128, 8, 128], F32, "stg") for _ in range(2)]
        wrw = al([128, 8, 896], BF16, "wrw")
        load_w_bf16(wrw, winv[:, :, C_RW:C_RW + 896], 896, stg, wrw.name)
        wup_f = TT(stg[0][:].rearrange("p a b -> p (a b)")[:, 0:768].rearrange("p (a b) -> p a b", b=256), stg[0].name); wup = al([128, 3, 256], BF16, "wup")
        P.dma("sp", lambda e: e.dma_start(out=wup_f[:], in_=dr["rw_upT"]), writes=[wup_f.name])
        P.op("pool", lambda e: e.tensor_copy(out=wup[:], in_=wup_f[:]), reads=[wup_f.name], writes=[wup.name])
        prm = al([128, 7, 2], F32, "rwprm")
        mu = al([128, 7], F32, "rwmu"); omm = al([128, 7], F32, "rwomm")
        P.dma("sp", lambda e: e.dma_start(out=prm[:], in_=dr["rw_prmT"]), writes=[prm.name])
        P.dma("sp", lambda e: e.dma_start(out=mu[:], in_=dr["rw_muT"]), writes=[mu.name])
        P.op("dve", lambda e: e.tensor_scalar(out=omm[:], in0=mu[:], scalar1=-1.0, scalar2=1.0, op0=ALU.mult, op1=ALU.add), reads=[mu.name], writes=[omm.name])
        cst_f = TT(stg[1][:].rearrange("p a b -> p (a b)")[:, 0:768].rearrange("p (a b) -> p a b", b=128), stg[1].name); cst = al([128, 6, 128], BF16, "rwcst")
        P.dma("sp", lambda e: e.dma_start(out=cst_f[:], in_=dr["rw_cst"]), writes=[cst_f.name])
        P.op("pool", lambda e: e.tensor_copy(out=cst[:], in_=cst_f[:]), reads=[cst_f.name], writes=[cst.name])
        hmask = al([128, 2], F32, "rwhmask")
        P.dma("sp", lambda e: e.dma_start(out=hmask[:], in_=dr["rw_hmask"]), writes=[hmask.name])
        ones64 = al([128, 2, LC], F32, "ones64")
        P.op("pool", lambda e: e.memset(ones64[:], 1.0), writes=[ones64.name])
        RB = 256
        fraw = al([128, 7, RB + 1], F32, "fraw"); Fs = al([128, 7, RB], F32, "Fs")
        P.op("pool", lambda e: e.memset(fraw[:, :, 0:1], 0.0), writes=[fraw.name])
        ft = {n_: al([128, 2, RB], F32, "rwf_" + n_) for n_ in ("logd", "a", "g", "kk", "kp", "nb", "bon")}
        ft["t1"] = ft["bon"]; ft["yfm"] = ft["a"]
        actb = al([128, 3, RB], BF16, "rwactb")
        t1b = al([128, 2, RB], BF16, "rwt1b")
        STf = al([128, 2, LC], F32, "STf"); STb = al([128, 2, LC], BF16, "STb")
        P.op("pool", lambda e: e.memset(STf[:], 0.0), writes=[STf.name])
        P.op("pool", lambda e: e.memset(STb[:], 0.0), writes=[STb.name])
        ck = {n_: al([128, 2, LC], F32, "rwc_" + n_) for n_ in ("lc", "c", "ci", "cp", "tmp")}
        cb = {n_: al([128, 2, LC], BF16, "rwb_" + n_) for n_ in ("kkt", "nbt", "kt", "rt", "v")}
        cm = {n_: al([128, 2, 2, LC], BF16, "rwm_" + n_) for n_ in ("kkt", "nbt", "kt", "rt")}
        cL = al([128, 2], F32, "rwcL")
        am = {n_: al([LC, 4, LC], BF16, "rwa_" + n_) for n_ in ("Mn", "MnT", "Nm", "Pn", "Q", "A", "AT", "Tb")}
        Tf = al([LC, 4, LC], F32, "rwTf")
        vT = al([LC, 2, 128], BF16, "rwvT"); trs = al([LC, 4, 128], BF16, "rwtrs")
        nbT = al([LC, 4, 128], BF16, "rwnbT"); ktT = al([LC, 4, 128], BF16, "rwktT")
        rhsT = al([LC, 4, LC], BF16, "rwrhsT"); uT = al([LC, 4, LC], BF16, "rwuT")
        yT = al([LC, 4, LC], F32, "rwyT"); ycen = al([LC, 4, LC], F32, "rwycen"); ysq = al([LC, 4, LC], F32, "rwysq"); ynb = al([LC, 4, LC], BF16, "rwynb")
        gst = {n_: al([LC, 4], F32, "rwg_" + n_) for n_ in ("sum", "var")}
        selt = al([128, 128], F32, "rwselt")
        identb64 = ident_b
        bk = [0]

        def bank2():
            bk[0] += 1
            return banks[bk[0] % 2]

        def rw_block(nb):
            sl = slice(nb * RB, (nb + 1) * RB)
            osl = slice(nb * (RB // 2), (nb + 1) * (RB // 2))
            for c7 in range(7):
                pa = bank2()
                for k in range(8):
                    P.op("pe", lambda e, k=k, pa=pa, c7=c7: e.matmul(pa[:, 0:RB], lhsT=wrw[:, k, c7 * 128:(c7 + 1) * 128], rhs=hT[:, k, sl], start=(k == 0), stop=(k == 7)),
                         reads=[wrw.name, hT.name], writes=[pa.name])
                P.op("act", lambda e, pa=pa, c7=c7: e.copy(out=fraw[:, c7, 1:RB + 1], in_=pa[:, 0:RB]), reads=[pa.name], writes=[fraw.name])
            for c7 in range(7):
                P.op("pool", lambda e, c7=c7: e.tensor_scalar(out=Fs[:, c7, :], in0=fraw[:, c7, 0:RB], scalar1=mu[:, c7:c7 + 1], scalar2=None, op0=ALU.mult), reads=[fraw.name, mu.name, Fs.name], writes=[Fs.name])
                P.op("dve", lambda e, c7=c7: e.scalar_tensor_tensor(out=Fs[:, c7, :], in0=fraw[:, c7, 1:RB + 1], scalar=omm[:, c7:c7 + 1], in1=Fs[:, c7, :], op0=ALU.mult, op1=ALU.add),
                     reads=[fraw.name, omm.name, Fs.name], writes=[Fs.name])
            P.op("dve", lambda e: e.tensor_copy(out=fraw[:, :, 0:1], in_=fraw[:, :, RB:RB + 1]), reads=[fraw.name, Fs.name], writes=[fraw.name])
            rF = Fs[:, 0:2, :]; kF = Fs[:, 2:4, :]; vF = Fs[:, 4:6, :]
            P.op("act", lambda e: e.activation(out=actb[:, 0, :], in_=Fs[:, 6, :], func=AF.Tanh), reads=[Fs.name], writes=[actb.name])
            P.op("act", lambda e: e.copy(out=actb[:, 1, :], in_=Fs[:, 6, :]), reads=[Fs.name], writes=[actb.name])
            P.op("act", lambda e: e.activation(out=actb[:, 2, :], in_=Fs[:, 6, :], func=AF.Sigmoid), reads=[Fs.name], writes=[actb.name])
            for ct in range(2):
                for wi, (dst, fn, pi) in enumerate((("logd", AF.Sigmoid, 0), ("a", AF.Sigmoid, 1), ("g", AF.Identity, None))):
                    pa = bank2()
                    P.op("pe", lambda e, pa=pa, wi=wi, ct=ct: e.matmul(pa[:, 0:RB], lhsT=wup[:, wi, ct * 128:(ct + 1) * 128], rhs=actb[:, wi, :], start=True, stop=True), reads=[wup.name, actb.name], writes=[pa.name])
                    if pi is None:
                        P.op("act", lambda e, pa=pa, ct=ct, dst=dst: e.copy(out=ft[dst][:, ct, :], in_=pa[:, 0:RB]), reads=[pa.name], writes=[ft[dst].name])
                    else:
                        P.op("act", lambda e, pa=pa, ct=ct, dst=dst, fn=fn, pi=pi: e.activation(out=ft[dst][:, ct, :], in_=pa[:, 0:RB], func=fn, bias=prm[:, pi, ct:ct + 1]), reads=[pa.name, prm.name], writes=[ft[dst].name])
            P.op("dve", lambda e: e.tensor_scalar(out=ft["logd"][:], in0=ft["logd"][:], scalar1=-math.exp(-0.5), scalar2=None, op0=ALU.mult), reads=[ft["logd"].name], writes=[ft["logd"].name])
            for ct in range(2):
                P.op("dve", lambda e, ct=ct: e.tensor_scalar(out=ft["kk"][:, ct, :], in0=kF[:, ct, :], scalar1=prm[:, 2, ct:ct + 1], scalar2=None, op0=ALU.mult), reads=[Fs.name, prm.name], writes=[ft["kk"].name])
                P.op("pool", lambda e, ct=ct: e.tensor_tensor(out=t1b[:, ct, :], in0=ft["kk"][:, ct, :], in1=ft["kk"][:, ct, :], op=ALU.mult), reads=[ft["kk"].name], writes=[t1b.name])
                pa = bank2()
                P.op("pe", lambda e, pa=pa, ct=ct: e.matmul(pa[:, 0:RB], lhsT=cst[:, 0, :], rhs=t1b[:, ct, :], start=True, stop=True), reads=[cst.name, t1b.name], writes=[pa.name])
                P.op("act", lambda e, pa=pa, ct=ct: e.activation(out=ft["t1"][:, ct, :], in_=pa[:, 0:RB], func=AF.Sqrt), reads=[pa.name], writes=[ft["t1"].name])
                P.op("dve", lambda e, ct=ct: e.tensor_scalar(out=ft["t1"][:, ct, :], in0=ft["t1"][:, ct, :], scalar1=1e-12, scalar2=None, op0=ALU.max), reads=[ft["t1"].name], writes=[ft["t1"].name])
                P.op("dve", lambda e, ct=ct: e.reciprocal(out=ft["t1"][:, ct, :], in_=ft["t1"][:, ct, :]), reads=[ft["t1"].name], writes=[ft["t1"].name])
                P.op("dve", lambda e, ct=ct: e.tensor_tensor(out=ft["kk"][:, ct, :], in0=ft["kk"][:, ct, :], in1=ft["t1"][:, ct, :], op=ALU.mult), reads=[ft["t1"].name, ft["kk"].name], writes=[ft["kk"].name])
                P.op("dve", lambda e, ct=ct: e.tensor_scalar(out=ft["kp"][:, ct, :], in0=ft["a"][:, ct, :], scalar1=-1.0, scalar2=prm[:, 3, ct:ct + 1], op0=ALU.add, op1=ALU.mult), reads=[ft["a"].name, prm.name], writes=[ft["kp"].name])
                P.op("dve", lambda e, ct=ct: e.scalar_tensor_tensor(out=ft["kp"][:, ct, :], in0=ft["kp"][:, ct, :], scalar=1.0, in1=kF[:, ct, :], op0=ALU.add, op1=ALU.mult), reads=[ft["kp"].name, Fs.name], writes=[ft["kp"].name])
                P.op("dve", lambda e, ct=ct: e.scalar_tensor_tensor(out=ft["nb"][:, ct, :], in0=ft["kk"][:, ct, :], scalar=-1.0, in1=ft["a"][:, ct, :], op0=ALU.mult, op1=ALU.mult), reads=[ft["kk"].name, ft["a"].name], writes=[ft["nb"].name])
                P.op("dve", lambda e, ct=ct: e.scalar_tensor_tensor(out=t1b[:, ct, :], in0=rF[:, ct, :], scalar=prm[:, 6, ct:ct + 1], in1=ft["kp"][:, ct, :], op0=ALU.mult, op1=ALU.mult), reads=[Fs.name, prm.name, ft["kp"].name, t1b.name], writes=[t1b.name])
                pa = bank2()
                P.op("pe", lambda e, pa=pa, ct=ct: e.matmul(pa[:, 0:RB], lhsT=cst[:, 0, :], rhs=t1b[:, ct, :], start=True, stop=True), reads=[cst.name, t1b.name], writes=[pa.name])
                P.op("dve", lambda e, pa=pa, ct=ct: e.tensor_tensor(out=ft["bon"][:, ct, :], in0=pa[:, 0:RB], in1=vF[:, ct, :], op=ALU.mult), reads=[pa.name, Fs.name], writes=[ft["bon"].name])
            for cc in range(RB // LC if RW_STOP != 1 else 0):
                rw_chunk(cc, rF, kF, vF)
            for ct in range(2):
                P.op("dve", lambda e, ct=ct: e.tensor_scalar(out=ft["yfm"][:, ct, :], in0=ft["yfm"][:, ct, :], scalar1=prm[:, 4, ct:ct + 1], scalar2=prm[:, 5, ct:ct + 1], op0=ALU.mult, op1=ALU.add), reads=[ft["yfm"].name, prm.name], writes=[ft["yfm"].name])
                P.op("dve", lambda e, ct=ct: e.tensor_tensor(out=ft["yfm"][:, ct, :], in0=ft["yfm"][:, ct, :], in1=ft["bon"][:, ct, :], op=ALU.add), reads=[ft["yfm"].name, ft["bon"].name], writes=[ft["yfm"].name])
                P.op("dve", lambda e, ct=ct: e.tensor_tensor(out=ft["yfm"][:, ct, :], in0=ft["yfm"][:, ct, :], in1=ft["g"][:, ct, :], op=ALU.mult), reads=[ft["yfm"].name, ft["g"].name], writes=[ft["yfm"].name])
                sv = ft["yfm"][:, ct, :].rearrange("p (a two c) -> p a two c", two=2, c=128)
                dv = br_own[:, 6 + ct, osl].rearrange("p (a c) -> p a c", c=128)
                tv = selt[:].rearrange("p (a c) -> p a c", c=128)
                P.op("pool", lambda e, sv=sv, tv=tv: e.tensor_scalar(out=tv, in0=sv[:, :, 0, :], scalar1=flags[:, 0:1], scalar2=None, op0=ALU.mult), reads=[ft["yfm"].name, flags.name, selt.name], writes=[selt.name])
                P.op("dve", lambda e, sv=sv, tv=tv, dv=dv: e.scalar_tensor_tensor(out=dv, in0=sv[:, :, 1, :], scalar=flags[:, 1:2], in1=tv, op0=ALU.mult, op1=ALU.add), reads=[ft["yfm"].name, selt.name, flags.name], writes=[br_own.name])

        def rw_chunk(cc, rF, kF, vF):
            cs = slice(cc * LC, (cc + 1) * LC)
            for ct in range(2):
                P.op("dve", lambda e, ct=ct: e.tensor_tensor_scan(out=ck["lc"][:, ct, :], data0=ones64[:, ct, :], data1=ft["logd"][:, ct, cs], initial=0.0, op0=ALU.mult, op1=ALU.add),
                     reads=[ones64.name, ft["logd"].name, ck["lc"].name], writes=[ck["lc"].name])
            P.op("act", lambda e: e.activation(out=ck["c"][:], in_=ck["lc"][:], func=AF.Exp), reads=[ck["lc"].name], writes=[ck["c"].name])
            P.op("act", lambda e: e.activation(out=ck["ci"][:], in_=ck["lc"][:], func=AF.Exp, scale=-1.0), reads=[ck["lc"].name], writes=[ck["ci"].name])
            P.op("dve", lambda e: e.tensor_tensor(out=ck["tmp"][:], in0=ck["lc"][:], in1=ft["logd"][:, :, cs], op=ALU.subtract), reads=[ck["lc"].name, ft["logd"].name], writes=[ck["tmp"].name])
            P.op("act", lambda e: e.activation(out=ck["cp"][:], in_=ck["tmp"][:], func=AF.Exp), reads=[ck["tmp"].name], writes=[ck["cp"].name])
            P.op("dve", lambda e: e.tensor_copy(out=cL[:], in_=ck["c"][:, :, LC - 1]), reads=[ck["c"].name], writes=[cL.name])
            for nm, src, fac in (("kkt", ft["kk"], "cp"), ("nbt", ft["nb"], "ci"), ("kt", ft["kp"], "ci")):
                P.op("pool", lambda e, nm=nm, src=src, fac=fac: e.tensor_tensor(out=cb[nm][:], in0=src[:, :, cs], in1=ck[fac][:], op=ALU.mult), reads=[src.name, ck[fac].name], writes=[cb[nm].name])
            P.op("pool", lambda e: e.tensor_tensor(out=cb["rt"][:], in0=rF[:, :, cs], in1=ck["c"][:], op=ALU.mult), reads=[Fs.name, ck["c"].name], writes=[cb["rt"].name])
            P.op("pool", lambda e: e.tensor_copy(out=cb["v"][:], in_=vF[:, :, cs]), reads=[Fs.name], writes=[cb["v"].name])
            for nm in ("kkt", "nbt", "kt", "rt"):
                for hh in range(2):
                    P.op("dve", lambda e, nm=nm, hh=hh: e.tensor_scalar(out=cm[nm][:, :, hh, :], in0=cb[nm][:], scalar1=hmask[:, hh:hh + 1], scalar2=None, op0=ALU.mult), reads=[cb[nm].name, hmask.name], writes=[cm[nm].name])
            specs = (("Mn", "nbt", "kkt", 1, 2), ("MnT", "kkt", "nbt", 3, 2), ("Nm", "kt", "kkt", 1, 3), ("Pn", "nbt", "rt", 2, 3), ("Q", "kt", "rt", 2, 4))
            for nm, ln, rn, mi, bi in specs:
                half = {"Mn": 0, "MnT": 1, "Nm": 0, "Pn": 1, "Q": 0}[nm]
                pb = banks[bi]
                for h in range(4):
                    P.op("pe", lambda e, pb=pb, h=h, ln=ln, rn=rn, half=half: e.matmul(pb[0:LC, half * 256 + h * LC:half * 256 + (h + 1) * LC], lhsT=cm[ln][:, h // 2, h % 2, :], rhs=cb[rn][:, h // 2, :], start=True, stop=True),
                         reads=[cm[ln].name, cb[rn].name], writes=[pb.name])
                P.op("dve", lambda e, pb=pb, nm=nm, mi=mi, half=half: e.tensor_tensor(out=am[nm][:], in0=pb[0:LC, half * 256:half * 256 + 256].rearrange("p (h t) -> p h t", t=LC),
                                                                                    in1=cst[0:LC, mi, 0:LC].unsqueeze(1).to_broadcast([LC, 4, LC]), op=ALU.mult), reads=[pb.name, cst.name], writes=[am[nm].name])
            if RW_STOP == 2:
                return
            P.op("dve", lambda e: e.tensor_tensor(out=Tf[:], in0=am["Mn"][:], in1=ident_f[0:LC, 0:LC].unsqueeze(1).to_broadcast([LC, 4, LC]), op=ALU.add), reads=[am["Mn"].name, ident_f.name, Tf.name], writes=[Tf.name])
            P.op("pool", lambda e: e.tensor_copy(out=am["Tb"][:], in_=Tf[:]), reads=[Tf.name], writes=[am["Tb"].name])
            curA, curAT = am["Mn"], am["MnT"]
            for lv in range(1, 6):
                pb = banks[5]
                for h in range(4):
                    if lv < 5:
                        P.op("pe", lambda e, pb=pb, h=h, curA=curA, curAT=curAT: e.matmul(pb[0:LC, h * LC:(h + 1) * LC], lhsT=curAT[:, h, :], rhs=curA[:, h, :], start=True, stop=True),
                             reads=[curA.name, curAT.name], writes=[pb.name])
                    P.op("pe", lambda e, pb=pb, h=h, curA=curA, curAT=curAT: e.matmul(pb[0:LC, 256 + h * LC:256 + (h + 1) * LC], lhsT=curA[:, h, :], rhs=curAT[:, h, :], start=True, stop=True),
                         reads=[curA.name, curAT.name], writes=[pb.name])
                if lv < 5:
                    P.op("act", lambda e, pb=pb: e.copy(out=am["A"][:], in_=pb[0:LC, 0:256].rearrange("p (h t) -> p h t", t=LC)), reads=[pb.name], writes=[am["A"].name])
                P.op("act", lambda e, pb=pb: e.copy(out=am["AT"][:], in_=pb[0:LC, 256:512].rearrange("p (h t) -> p h t", t=LC)), reads=[pb.name], writes=[am["AT"].name])
                curA, curAT = am["A"], am["AT"]
                pt = banks[6]
                for h in range(4):
                    P.op("pe", lambda e, pt=pt, h=h: e.matmul(pt[0:LC, h * LC:(h + 1) * LC], lhsT=am["AT"][:, h, :], rhs=am["Tb"][:, h, :], start=True, stop=True), reads=[am["AT"].name, am["Tb"].name], writes=[pt.name])
                P.op("dve", lambda e, pt=pt: e.tensor_tensor(out=Tf[:], in0=Tf[:], in1=pt[0:LC, 0:256].rearrange("p (h t) -> p h t", t=LC), op=ALU.add), reads=[pt.name, Tf.name], writes=[Tf.name])
                P.op("pool", lambda e: e.tensor_copy(out=am["Tb"][:], in_=Tf[:]), reads=[Tf.name], writes=[am["Tb"].name])
            if RW_STOP == 3:
                return
            tcol = 0
            tsl = {}
            for nm in ("v", "nbt", "kt"):
                for ct in range(2):
                    P.op("pe", lambda e, nm=nm, ct=ct, tcol=tcol: e.transpose(out=bank_bf[0:LC, tcol:tcol + 128], in_=cb[nm][:, ct, :], identity=ident_b[:]), reads=[cb[nm].name, ident_b.name], writes=["bank7"])
                    tsl[(nm, ct)] = slice(tcol, tcol + 128)
                    tcol += 128
            if RW_STOP == 41:
                return
            P.op("act", lambda e: e.copy(out=vT[:], in_=bank_bf[0:LC, 0:256].rearrange("p (c x) -> p c x", x=128)), reads=["bank7"], writes=[vT.name])
            if RW_STOP == 42:
                return
            P.op("act", lambda e: e.copy(out=trs[:], in_=bank_bf[0:LC, 256:768].rearrange("p (c x) -> p c x", x=128)), reads=["bank7"], writes=[trs.name])
            for h in range(4):
                P.op("pool", lambda e, h=h: e.tensor_tensor(out=nbT[:, h, :], in0=trs[:, h // 2, :], in1=cst[0:LC, 4 + h % 2, :], op=ALU.mult), reads=[trs.name, cst.name], writes=[nbT.name])
                P.op("pool", lambda e, h=h: e.tensor_tensor(out=ktT[:, h, :], in0=trs[:, 2 + h // 2, :], in1=cst[0:LC, 4 + h % 2, :], op=ALU.mult), reads=[trs.name, cst.name], writes=[ktT.name])
            if RW_STOP == 4:
                return
            pr = banks[0]
            for h in range(4):
                P.op("pe", lambda e, pr=pr, h=h: e.matmul(pr[0:LC, h * LC:(h + 1) * LC], lhsT=cm["kkt"][:, h // 2, h % 2, :], rhs=STb[:, h // 2, :], start=True, stop=False), reads=[cm["kkt"].name, STb.name], writes=[pr.name])
                P.op("pe", lambda e, pr=pr, h=h: e.matmul(pr[0:LC, h * LC:(h + 1) * LC], lhsT=am["Nm"][:, h, :], rhs=vT[:, h // 2, (h % 2) * LC:(h % 2 + 1) * LC], start=False, stop=True), reads=[am["Nm"].name, vT.name], writes=[pr.name])
            P.op("act", lambda e, pr=pr: e.copy(out=rhsT[:], in_=pr[0:LC, 0:256].rearrange("p (h t) -> p h t", t=LC)), reads=[pr.name], writes=[rhsT.name])
            pu_ = banks[1]
            for h in range(4):
                P.op("pe", lambda e, pu_=pu_, h=h: e.matmul(pu_[0:LC, h * LC:(h + 1) * LC], lhsT=am["Tb"][:, h, :], rhs=rhsT[:, h, :], start=True, stop=True), reads=[am["Tb"].name, rhsT.name], writes=[pu_.name])
            P.op("act", lambda e, pu_=pu_: e.copy(out=uT[:], in_=pu_[0:LC, 0:256].rearrange("p (h t) -> p h t", t=LC)), reads=[pu_.name], writes=[uT.name])
            py_ = banks[2]
            for h in range(4):
                P.op("pe", lambda e, h=h: e.matmul(py_[0:LC, h * LC:(h + 1) * LC], lhsT=cm["rt"][:, h // 2, h % 2, :], rhs=STb[:, h // 2, :], start=True, stop=False), reads=[cm["rt"].name, STb.name], writes=[py_.name])
                P.op("pe", lambda e, h=h: e.matmul(py_[0:LC, h * LC:(h + 1) * LC], lhsT=am["Pn"][:, h, :], rhs=uT[:, h, :], start=False, stop=False), reads=[am["Pn"].name, uT.name], writes=[py_.name])
                P.op("pe", lambda e, h=h: e.matmul(py_[0:LC, h * LC:(h + 1) * LC], lhsT=am["Q"][:, h, :], rhs=vT[:, h // 2, (h % 2) * LC:(h % 2 + 1) * LC], start=False, stop=True), reads=[am["Q"].name, vT.name], writes=[py_.name])
            P.op("act", lambda e: e.copy(out=yT[:], in_=py_[0:LC, 0:256].rearrange("p (h t) -> p h t", t=LC)), reads=[py_.name], writes=[yT.name])
            if RW_STOP == 5:
                return
            ps_ = banks[3]
            for ct in range(2):
                for hh in range(2):
                    h = 2 * ct + hh
                    P.op("pe", lambda e, ct=ct, h=h: e.matmul(ps_[:, ct * LC:(ct + 1) * LC], lhsT=nbT[:, h, :], rhs=uT[:, h, :], start=(h % 2 == 0), stop=False), reads=[nbT.name, uT.name], writes=[ps_.name])
                    P.op("pe", lambda e, ct=ct, h=h, hh=hh: e.matmul(ps_[:, ct * LC:(ct + 1) * LC], lhsT=ktT[:, h, :], rhs=vT[:, ct, hh * LC:(hh + 1) * LC], start=False, stop=(hh == 1)), reads=[ktT.name, vT.name], writes=[ps_.name])
            for ct in range(2):
                P.op("dve", lambda e, ct=ct: e.tensor_scalar(out=STf[:, ct, :], in0=STf[:, ct, :], scalar1=cL[:, ct:ct + 1], scalar2=None, op0=ALU.mult), reads=[cL.name, STf.name, STb.name], writes=[STf.name])
                P.op("dve", lambda e, ct=ct: e.scalar_tensor_tensor(out=STf[:, ct, :], in0=ps_[:, ct * LC:(ct + 1) * LC], scalar=cL[:, ct:ct + 1], in1=STf[:, ct, :], op0=ALU.mult, op1=ALU.add), reads=[ps_.name, cL.name, STf.name], writes=[STf.name])
            P.op("pool", lambda e: e.tensor_copy(out=STb[:], in_=STf[:]), reads=[STf.name], writes=[STb.name])
            if RW_STOP == 6:
                return
            AXn = mybir.AxisListType.X
            P.op("dve", lambda e: e.tensor_reduce(out=gst["sum"][:], in_=yT[:], axis=AXn, op=ALU.add), reads=[yT.name], writes=[gst["sum"].name])
            P.op("dve", lambda e: e.tensor_scalar(out=gst["sum"][:], in0=gst["sum"][:], scalar1=-1.0 / LC, scalar2=None, op0=ALU.mult), reads=[gst["sum"].name], writes=[gst["sum"].name])
            P.op("dve", lambda e: e.tensor_tensor(out=ycen[:], in0=yT[:], in1=gst["sum"][:].unsqueeze(2).to_broadcast([LC, 4, LC]), op=ALU.add), reads=[yT.name, gst["sum"].name], writes=[ycen.name])
            P.op("pool", lambda e: e.tensor_tensor(out=ysq[:], in0=ycen[:], in1=ycen[:], op=ALU.mult), reads=[ycen.name], writes=[ysq.name])
            P.op("dve", lambda e: e.tensor_reduce(out=gst["var"][:], in_=ysq[:], axis=AXn, op=ALU.add), reads=[ysq.name], writes=[gst["var"].name])
            P.op("act", lambda e: e.activation(out=gst["var"][:], in_=gst["var"][:], func=AF.Sqrt, scale=1.0 / LC, bias=gneps[0:LC, :]), reads=[gst["var"].name, gneps.name], writes=[gst["var"].name])
            P.op("dve", lambda e: e.reciprocal(out=gst["var"][:], in_=gst["var"][:]), reads=[gst["var"].name], writes=[gst["var"].name])
            P.op("dve", lambda e: e.tensor_tensor(out=ynb[:], in0=ycen[:], in1=gst["var"][:].unsqueeze(2).to_broadcast([LC, 4, LC]), op=ALU.mult), reads=[ycen.name, gst["var"].name], writes=[ynb.name])
            for ct in range(2):
                P.op("pe", lambda e, ct=ct: e.transpose(out=bank_bf[:, 768 + ct * LC:768 + (ct + 1) * LC], in_=ynb[:, 2 * ct:2 * ct + 2, :].rearrange("p h v -> p (h v)"), identity=ident_b[0:LC, 0:LC]),
                     reads=[ynb.name, ident_b.name], writes=["bank7y"])
            P.op("act", lambda e: e.copy(out=ft["yfm"][:, :, cs], in_=bank_bf[:, 768:768 + 2 * LC].rearrange("p (c t) -> p c t", t=LC)), reads=["bank7y"], writes=[ft["yfm"].name])

        gneps = al([128, 1], F32, "gneps")
        P.op("pool", lambda e: e.memset(gneps[:], 64e-5), writes=[gneps.name])
        for nb in range(T // RB):
            rw_block(nb)
        P.emit()

    if branches[1]:
        NT = T // 128
        NOT = TO // 128
        al = Bump(R2, REND)
        KT = al([128, 2, T], BF16, "KT")
        ik4 = al([128, T], BF16, "ik4")
        Vtm = al([128, NT, 4, 66], BF16, "Vtm")
        Qo = al([128, 2, TO], BF16, "Qo")
        iqo = al([128, TO], BF16, "iqo")
        iwf = al([128, NT, 4], F32, "iwf")
        iwo = al([128, NOT, 4], F32, "iwo")
        mark = al.off
        stg = [al([128, 8, 128], F32, "stg") for _ in range(2)]
        wqk = al([128, 8, 896], BF16, "wqk")
        wik = al([128, 8, 128], BF16, "wik")
        wiw = al([128, 8, 4], BF16, "wiw")
        load_w_bf16(wqk, winv[:, :, C_Q:C_Q + 896], 896, stg, wqk.name)
        load_w_bf16(wik, dr["w_ik4"].rearrange("(k p) n -> p k n", p=128), 128, stg, wik.name)
        stg4 = al([128, 8, 4], F32, "stg4")
        P.dma("sp", lambda e: e.dma_start(out=stg4[:], in_=winv[:, :, C_IW:C_IW + 4]), writes=[stg4.name])
        P.op("pool", lambda e: e.tensor_copy(out=wiw[:], in_=stg4[:]), reads=[stg4.name], writes=[wiw.name])
        ropec = al([128, 4], F32, "ropec")
        pm_f = al([128, 2, 128], F32, "pm_f"); pmb = al([128, 2, 128], BF16, "pmb")
        P.dma("sp", lambda e: e.dma_start(out=ropec[:], in_=dr["rope_c"]), writes=[ropec.name])
        P.dma("sp", lambda e: e.dma_start(out=pm_f[:, 0, :], in_=dr["perm64"]), writes=[pm_f.name])
        P.dma("sp", lambda e: e.dma_start(out=pm_f[:, 1, :], in_=dr["perm32"]), writes=[pm_f.name])
        P.op("pool", lambda e: e.tensor_copy(out=pmb[:], in_=pm_f[:]), reads=[pm_f.name], writes=[pmb.name])
        P.op("pool", lambda e: e.memset(Vtm[:], 1.0), writes=[Vtm.name])
        posi = al([128, 512], I32, "posi"); posf = al([128, 512], F32, "posf")
        tb = {n_: al([128, 512], F32, "rp_" + n_) for n_ in ("y", "wf", "wm", "c64", "s64", "c32", "s32")}
        tb["t1"], tb["t2"], tb["rot"] = tb["y"], tb["wf"], tb["wm"]
        twi = al([128, 512], I32, "rp_wi")
        xsb = al([128, 512], BF16, "xsb")
        selt = al([128, 256], F32, "selt")
        RK = "ropetab"
        bi_ = [0]

        def bank():
            bi_[0] += 1
            return banks[bi_[0] % 7]

        def sel_blk(dst, src, key_src, key_dst):
            sv = src.rearrange("p (a two c) -> p a two c", two=2, c=128)
            dv = dst.rearrange("p (a c) -> p a c", c=128)
            tv = selt[:].rearrange("p (a c) -> p a c", c=128)
            P.op("pool", lambda e: e.tensor_scalar(out=tv, in0=sv[:, :, 0, :], scalar1=flags[:, 0:1], scalar2=None, op0=ALU.mult), reads=[key_src, flags.name], writes=[selt.name])
            P.op("dve", lambda e: e.scalar_tensor_tensor(out=dv, in0=sv[:, :, 1, :], scalar=flags[:, 1:2], in1=tv, op0=ALU.mult, op1=ALU.add),
                 reads=[key_src, selt.name, flags.name], writes=[key_dst])

        def dsa_blk(nb):
            sl = slice(nb * 512, (nb + 1) * 512)
            P.dma("sp", lambda e, sl=sl: e.dma_start(out=posi[:], in_=dr["pos"][0, sl].partition_broadcast(128)), writes=[posi.name])
            P.op("dve", lambda e: e.tensor_copy(out=posf[:], in_=posi[:]), reads=[posi.name], writes=[posf.name])
            for ci, (cn, sn) in enumerate((("c64", "s64"), ("c32", "s32"))):
                P.op("dve", lambda e, ci=ci: e.tensor_scalar(out=tb["y"][:], in0=posf[:], scalar1=ropec[:, 2 * ci:2 * ci + 1], scalar2=None, op0=ALU.mult),
                     reads=[posf.name, ropec.name, RK, "sc_out", "rp_t1", "rp_t2", "rp_rot"], writes=[RK, "sc_out", "rp_t1", "rp_t2", "rp_rot", "sc_wf", "sc_wm", "ropegate"])
                sin_2pi(tb["y"][:], tb[sn][:], twi[:], tb["wf"][:], tb["wm"][:], RK, 512)
                P.op("dve", lambda e: e.tensor_scalar(out=tb["y"][:], in0=tb["y"][:], scalar1=0.25, scalar2=None, op0=ALU.add), reads=[RK, "sc_out"], writes=[RK])
                sin_2pi(tb["y"][:], tb[cn][:], twi[:], tb["wf"][:], tb["wm"][:], RK, 512)
                P.op("dve", lambda e, ci=ci, sn=sn: e.tensor_scalar(out=tb[sn][:], in0=tb[sn][:], scalar1=ropec[:, 2 * ci + 1:2 * ci + 2], scalar2=None, op0=ALU.mult),
                     reads=[RK, "sc_out", ropec.name], writes=[RK, "sc_out", "ropegate"])

            def proj_rope(wt, c0, ci, dst, dkey, own):
                px = bank(); pp_ = bank()
                for k in range(8):
                    P.op("pe", lambda e, k=k, px=px: e.matmul(px[:], lhsT=wt[:, k, c0:c0 + 128], rhs=hT[:, k, sl], start=(k == 0), stop=(k == 7)),
                         reads=[wt.name, hT.name], writes=[px.name])
                P.op("act", lambda e, px=px: e.copy(out=xsb[:], in_=px[:]), reads=[px.name], writes=[xsb.name])
                P.op("pe", lambda e, pp_=pp_: e.matmul(pp_[:], lhsT=pmb[:, ci, :], rhs=xsb[:], start=True, stop=True), reads=[pmb.name, xsb.name], writes=[pp_.name])
                cn, sn = ("c64", "s64") if ci == 0 else ("c32", "s32")
                P.op("dve", lambda e, px=px: e.tensor_tensor(out=tb["t1"][:], in0=px[:], in1=tb[cn][:], op=ALU.mult), reads=[px.name, "ropegate"], writes=["rp_t1"])
                P.op("dve", lambda e, pp_=pp_: e.tensor_tensor(out=tb["t2"][:], in0=pp_[:], in1=tb[sn][:], op=ALU.mult), reads=[pp_.name, "ropegate"], writes=["rp_t2"])
                if own:
                    P.op("pool", lambda e: e.tensor_tensor(out=tb["rot"][:], in0=tb["t1"][:], in1=tb["t2"][:], op=ALU.add), reads=["rp_t1", "rp_t2"], writes=["rp_rot"])
                    sel_blk(dst, tb["rot"][:], "rp_rot", dkey)
                else:
                    P.op("pool", lambda e: e.tensor_tensor(out=dst, in0=tb["t1"][:], in1=tb["t2"][:], op=ALU.add), reads=["rp_t1", "rp_t2"], writes=[dkey])

            osl = slice(nb * 256, (nb + 1) * 256)
            for ct in range(2):
                proj_rope(wqk, ct * 128, 0, Qo[:, ct, osl], Qo.name, True)
                proj_rope(wqk, 256 + ct * 128, 0, KT[:, ct, sl], KT.name, False)
            proj_rope(wqk, 768, 1, iqo[:, osl], iqo.name, True)
            proj_rope(wik, 0, 1, ik4[:, sl], ik4.name, False)
            for tt in range(4):
                tok = nb * 4 + tt
                tsl = slice(tok * 128, (tok + 1) * 128)
                pv = bank()
                for k in range(8):
                    P.op("pe", lambda e, k=k, pv=pv, tsl=tsl: e.matmul(pv[:, 0:256], lhsT=hT[:, k, tsl], rhs=wqk[:, k, 512:768], start=(k == 0), stop=(k == 7)),
                         reads=[wqk.name, hT.name], writes=[pv.name])
                for k in range(8):
                    P.op("pe", lambda e, k=k, pv=pv, tsl=tsl: e.matmul(pv[:, 256:260], lhsT=hT[:, k, tsl], rhs=wiw[:, k, :], start=(k == 0), stop=(k == 7)),
                         reads=[wiw.name, hT.name], writes=[pv.name])
                P.op("act", lambda e, pv=pv, tok=tok: e.copy(out=Vtm[:, tok, :, 0:64], in_=pv[:, 0:256].rearrange("p (h d) -> p h d", d=64)), reads=[pv.name], writes=[Vtm.name])
                P.op("act", lambda e, pv=pv, tok=tok: e.copy(out=iwf[:, tok, :], in_=pv[:, 256:260]), reads=[pv.name], writes=[iwf.name])

        for nb in range(NB):
            dsa_blk(nb)
        iwv = iwf[:].rearrange("p (a two) h -> p a two h", two=2)
        P.op("dve", lambda e: e.tensor_scalar(out=iwo[:], in0=iwv[:, :, 0, :], scalar1=flags[:, 0:1], scalar2=None, op0=ALU.mult), reads=[iwf.name, flags.name], writes=[iwo.name])
        P.op("dve", lambda e: e.scalar_tensor_tensor(out=iwo[:], in0=iwv[:, :, 1, :], scalar=flags[:, 1:2], in1=iwo[:], op0=ALU.mult, op1=ALU.add), reads=[iwf.name, flags.name, iwo.name], writes=[iwo.name])
        P.emit()

        al_s3 = Bump(mark, REND)
        Qm = [al_s3([128, 2, TO], BF16, "Qm%d" % a_) for a_ in range(2)]
        for a_ in range(2):
            P.op("pool", lambda e, a_=a_: e.tensor_copy(out=Qm[a_][:], in_=Qo[:]), reads=[Qo.name], writes=[Qm[a_].name])
            P.op("pool", lambda e, a_=a_: e.memset(Qm[a_][64 * (1 - a_):64 * (1 - a_) + 64, :, :], 0.0), reads=[Qm[a_].name], writes=[Qm[a_].name])
        iqo3 = al_s3([128, TO], BF16, "iqo3")
        P.op("pool", lambda e: e.tensor_copy(out=iqo3[64:128, :], in_=iqo[64:128, :]), reads=[iqo.name], writes=[iqo3.name])
        P.op("pool", lambda e: e.memset(iqo3[64:96, :], 0.0), reads=[iqo3.name], writes=[iqo3.name])
        al = Bump(R0, R1)
        tri = al([128, 128], F32, "tri"); bA = al([128, 128], F32, "bA"); bB = al([128, 128], F32, "bB"); negb = al([128, 1], F32, "negb")
        P.op("pool", lambda e: e.memset(tri[:], 0.0), writes=[tri.name])
        P.op("pool", lambda e: e.affine_select(out=tri[:], in_=tri[:], pattern=[[-1, 128]], compare_op=ALU.is_ge, fill=-BIG, base=0, channel_multiplier=1), reads=[tri.name], writes=[tri.name])
        P.op("dve", lambda e: e.tensor_scalar(out=bA[:], in0=tri[:], scalar1=flags[:, 0:1], scalar2=None, op0=ALU.mult), reads=[tri.name, flags.name], writes=[bA.name])
        P.op("dve", lambda e: e.tensor_scalar(out=negb[:], in0=flags[:, 0:1], scalar1=-BIG, scalar2=None, op0=ALU.mult), reads=[flags.name], writes=[negb.name])
        P.op("dve", lambda e: e.tensor_scalar(out=bB[:], in0=tri[:], scalar1=flags[:, 1:2], scalar2=negb[:, 0:1], op0=ALU.mult, op1=ALU.add), reads=[tri.name, flags.name, negb.name], writes=[bB.name])
        sc = al([128, T], F32, "sc"); wk = al([128, T], F32, "wk"); selm = al([128, T], BF16, "selm")
        rl = [al([128, 512], F32, "rl") for _ in range(2)]
        m8 = al([128, 8], F32, "m8"); thr = al([128, 1], F32, "thr")
        selT = [al([128, 128], BF16, "selT") for _ in range(2)]
        pT = [al([128, 4, 128], BF16, "pT") for _ in range(2)]
        rden = al([128, 4], F32, "rden"); ob = al([128, 4, 64], BF16, "ob")
        ps_sc = [banks[0], banks[0]]
        ps_sel = [TT(bank_bf[:, 0:128], "bank7a"), TT(bank_bf[:, 128:256], "bank7b")]
        ps_st = [banks[3], banks[6]]
        ps_o = [banks[4], banks[5]]
        ps_ot = TT(bank_bf[:, 512:768], "bank7c")
        oacc = al([128, 264], F32, "oacc")
        cnt_ = {"hi": 0, "ki": 0}

        def dsa_qtile(i):
            hi = cnt_["hi"]; ki = cnt_["ki"]
            S = (2 * i + 2) * 128
            qsl = slice(i * 128, (i + 1) * 128)
            for c0 in range(0, S, 512):
                cw = min(512, S - c0)
                csl = slice(c0, c0 + cw)
                for h in range(4):
                    pb = ps_sc[hi % 2]; r_ = rl[hi % 2]; hi += 1
                    if h < 3:
                        P.op("pe", lambda e, pb=pb, h=h, csl=csl, cw=cw, qsl=qsl: e.matmul(pb[:, 0:cw], lhsT=iqo[32 * h:32 * h + 32, qsl], rhs=ik4[32 * h:32 * h + 32, csl], start=True, stop=True),
                             reads=[iqo.name, ik4.name], writes=[pb.name])
                    else:
                        P.op("pe", lambda e, pb=pb, csl=csl, cw=cw, qsl=qsl: e.matmul(pb[:, 0:cw], lhsT=iqo3[64:128, qsl], rhs=ik4[64:128, csl], start=True, stop=True),
                             reads=[iqo3.name, ik4.name], writes=[pb.name])
                    if h == 0:
                        P.op("dve", lambda e, pb=pb, csl=csl, cw=cw, i=i: e.tensor_scalar(out=sc[:, csl], in0=pb[:, 0:cw], scalar1=0.0, scalar2=iwo[:, i, 0:1], op0=ALU.max, op1=ALU.mult),
                             reads=[pb.name, iwo.name, selm.name], writes=[sc.name])
                    else:
                        P.op("act", lambda e, pb=pb, r_=r_, cw=cw: e.activation(out=r_[:, 0:cw], in_=pb[:, 0:cw], func=AF.Relu), reads=[pb.name], writes=[r_.name])
                        P.op("dve", lambda e, r_=r_, csl=csl, cw=cw, i=i, h=h: e.scalar_tensor_tensor(out=sc[:, csl], in0=r_[:, 0:cw], scalar=iwo[:, i, h:h + 1], in1=sc[:, csl], op0=ALU.mult, op1=ALU.add),
                             reads=[r_.name, iwo.name, sc.name], writes=[sc.name])
            P.op("dve", lambda e, S=S: e.tensor_tensor(out=sc[:, S - 256:S - 128], in0=sc[:, S - 256:S - 128], in1=bA[:], op=ALU.add), reads=[sc.name, bA.name], writes=[sc.name])
            P.op("dve", lambda e, S=S: e.tensor_tensor(out=sc[:, S - 128:S], in0=sc[:, S - 128:S], in1=bB[:], op=ALU.add), reads=[sc.name, bB.name], writes=[sc.name])
            if S <= 256:
                P.op("dve", lambda e: e.memset(thr[:], -1.0e29), writes=[thr.name])
            else:
                for r in range(32):
                    src = sc if r == 0 else wk
                    P.op("dve", lambda e, src=src, S=S: e.max(out=m8[:], in_=src[:, 0:S]), reads=[src.name], writes=[m8.name])
                    if r < 31:
                        P.op("dve", lambda e, src=src, S=S: e.match_replace(out=wk[:, 0:S], in_to_replace=m8[:], in_values=src[:, 0:S], imm_value=-3.0e38), reads=[src.name, m8.name], writes=[wk.name])
                P.op("dve", lambda e: e.tensor_scalar(out=thr[:], in0=m8[:, 7:8], scalar1=-1.0e29, scalar2=None, op0=ALU.max), reads=[m8.name], writes=[thr.name])
            P.op("dve", lambda e, S=S: e.tensor_scalar(out=selm[:, 0:S], in0=sc[:, 0:S], scalar1=thr[:, 0:1], scalar2=None, op0=ALU.is_ge), reads=[sc.name, thr.name], writes=[selm.name])
            nkt = 2 * i + 2
            if DSA_STOP == 2:
                nkt = 0
            for kt in range(nkt):
                ksl = slice(kt * 128, (kt + 1) * 128)
                pse = ps_sel[ki % 2]; pst = ps_st[ki % 2]; sT = selT[ki % 2]; pT_ = pT[ki % 2]; ki += 1
                P.op("pe", lambda e, pse=pse, ksl=ksl: e.transpose(out=pse[:], in_=selm[:, ksl], identity=ident_b[:]), reads=[selm.name, ident_b.name], writes=[pse.name])
                P.op("act", lambda e, pse=pse, sT=sT: e.copy(out=sT[:], in_=pse[:]), reads=[pse.name], writes=[sT.name])
                for h in range(4):
                    hp = slice(64 * (h % 2), 64 * (h % 2) + 64)
                    P.op("pe", lambda e, pst=pst, h=h, ksl=ksl, qsl=qsl: e.matmul(pst[:, h * 128:(h + 1) * 128], lhsT=KT[:, h // 2, ksl], rhs=Qm[h % 2][:, h // 2, qsl], start=True, stop=True),
                         reads=[KT.name, Qm[h % 2].name], writes=[pst.name])
                P.op("act", lambda e, pst=pst, pT_=pT_: e.activation(out=pT_[:].rearrange("p h q -> p (h q)"), in_=pst[:], func=AF.Exp, scale=0.125), reads=[pst.name], writes=[pT_.name])
                P.op("dve", lambda e, pT_=pT_, sT=sT: e.tensor_tensor(out=pT_[:], in0=pT_[:], in1=sT[:].unsqueeze(1).to_broadcast([128, 4, 128]), op=ALU.mult), reads=[pT_.name, sT.name], writes=[pT_.name])
                pso = ps_o[kt % 2]
                for h in range(4):
                    P.op("pe", lambda e, h=h, pT_=pT_, kt=kt, pso=pso: e.matmul(pso[:, h * 66:(h + 1) * 66], lhsT=pT_[:, h, :], rhs=Vtm[:, kt, h, :], start=True, stop=True),
                         reads=[pT_.name, Vtm.name], writes=[pso.name])
                if kt == 0:
                    P.op("dve", lambda e, pso=pso: e.tensor_copy(out=oacc[:], in_=pso[:, 0:264]), reads=[pso.name, oacc.name], writes=[oacc.name])
                else:
                    P.op("dve", lambda e, pso=pso: e.tensor_tensor(out=oacc[:], in0=oacc[:], in1=pso[:, 0:264], op=ALU.add), reads=[pso.name, oacc.name], writes=[oacc.name])
            if DSA_STOP in (2, 3):
                cnt_["hi"] = hi; cnt_["ki"] = ki
                return
            pov = oacc[:].rearrange("p (h d) -> p h d", d=66)
            P.op("dve", lambda e: e.reciprocal(out=rden[:], in_=pov[:, :, 64]), reads=[oacc.name], writes=[rden.name])
            P.op("dve", lambda e: e.tensor_tensor(out=ob[:], in0=pov[:, :, 0:64], in1=rden[:].unsqueeze(2).to_broadcast([128, 4, 64]), op=ALU.mult), reads=[oacc.name, rden.name], writes=[ob.name])
            obf = ob[:].rearrange("p h d -> p (h d)")
            for ct in range(2):
                P.op("pe", lambda e, ct=ct: e.transpose(out=ps_ot[:, ct * 128:(ct + 1) * 128], in_=obf[:, ct * 128:(ct + 1) * 128], identity=ident_b[:]), reads=[ob.name, ident_b.name], writes=[ps_ot.name])
            P.op("act", lambda e, qsl=qsl: e.copy(out=br_own[:, 2:4, qsl], in_=ps_ot[:].rearrange("p (c q) -> p c q", q=128)), reads=[ps_ot.name], writes=[br_own.name])
            cnt_["hi"] = hi; cnt_["ki"] = ki

        for i in range(NOT if DSA_STOP != 1 else 0):
            dsa_qtile(i)
        P.emit()

    xo = Bump(R2, R2 + 64 * KB)([128, 8, TO], F32, "xo")
    hO = Bump(R0, R0 + 32 * KB)([128, 8, TO], BF16, "hO")
    pmx = Bump(R0 + 32 * KB, R1)([128, 8, TO], BF16, "pmx")
    WS = R2 + 64 * KB
    xov = dr["xoT"].rearrange("(k p) t -> p k t", p=128)
    for k in range(8):
        P.dma("sp" if k % 2 == 0 else "pool", lambda e, k=k: e.dma_start(out=xo[:, k, :], in_=xov[:, k, :]), writes=[xo.name])
    al = Bump(WS, REND)
    sq = al([128, 8, 512], F32, "sq"); rstd = al([128, 512], F32, "rstd")
    for ob in range(NOB):
        norm_block(xo[:, :, ob * 512:(ob + 1) * 512], xo.name, A1, mods[:, 0, :], sq, banks[ob % 2], rstd, out_bf=hO[:, :, ob * 512:(ob + 1) * 512], okb=hO.name)
    P.emit()
    al = Bump(WS, REND)
    gbT = al([128, 32], F32, "gbT")
    P.dma("sp", lambda e: e.dma_start(out=gbT[:], in_=dr["gate_bT"]), writes=[gbT.name])
    stg = [al([128, 8, 128], F32, "gstg") for _ in range(2)]
    stgb = [al([128, 2, 128], F32, "bstg") for _ in range(2)]
    wg = [al([128, 4, 8, 128], BF16, "wg") for _ in range(2)]
    wb = [al([128, 4, 2, 128], BF16, "wb") for _ in range(2)]
    G = [al([128, 512], F32, "G") for _ in range(2)]
    acc = [al([128, 512], F32, "acc") for _ in range(2)]
    pg = banks[0:2]; pu = banks[2:4]
    gwv = dr["gate_w"].rearrange("(k p) n -> p k n", p=128)
    si = 0
    it = 0
    for dt in range(8):
        wg_, wb_ = wg[dt % 2], wb[dt % 2]
        for n in range(4):
            st = stg[si % 2]; stb = stgb[si % 2]; si += 1
            c0 = n * D + dt * 128
            P.dma("sp", lambda e, st=st, c0=c0: e.dma_start(out=st[:], in_=gwv[:, :, c0:c0 + 128]), writes=[st.name])
            P.op("pool", lambda e, st=st, wg_=wg_, n=n: e.tensor_copy(out=wg_[:, n, :, :], in_=st[:]), reads=[st.name], writes=[wg_.name])
            P.dma("sp", lambda e, stb=stb, n=n, dt=dt: e.dma_start(out=stb[:], in_=dr["branch_w"][n].rearrange("(k p) d -> p k d", p=128)[:, :, dt * 128:(dt + 1) * 128]), writes=[stb.name])
            P.op("pool", lambda e, stb=stb, wb_=wb_, n=n: e.tensor_copy(out=wb_[:, n, :, :], in_=stb[:]), reads=[stb.name], writes=[wb_.name])
        for ob in range(NOB):
            sl = slice(ob * 512, (ob + 1) * 512)
            ac = acc[ob % 2]
            for n in range(4):
                pg_, pu_, G_ = pg[it % 2], pu[it % 2], G[it % 2]
                it += 1
                for k in range(8):
                    P.op("pe", lambda e, k=k, pg_=pg_, wg_=wg_, n=n, sl=sl: e.matmul(pg_[:], lhsT=wg_[:, n, k, :], rhs=hO[:, k, sl], start=(k == 0), stop=(k == 7)),
                         reads=[wg_.name, hO.name], writes=[pg_.name])
                for k in range(2):
                    P.op("pe", lambda e, k=k, pu_=pu_, wb_=wb_, n=n, sl=sl: e.matmul(pu_[:], lhsT=wb_[:, n, k, :], rhs=br_own[:, 2 * n + k, sl], start=(k == 0), stop=(k == 1)),
                         reads=[wb_.name, br_own.name], writes=[pu_.name])
                P.op("act", lambda e, pg_=pg_, G_=G_, n=n, dt=dt: e.activation(out=G_[:], in_=pg_[:], func=AF.Sigmoid, bias=gbT[:, n * 8 + dt:n * 8 + dt + 1]),
                     reads=[pg_.name, gbT.name], writes=[G_.name])
                if n == 0:
                    P.op("dve", lambda e, ac=ac, G_=G_, pu_=pu_: e.tensor_tensor(out=ac[:], in0=G_[:], in1=pu_[:], op=ALU.mult), reads=[G_.name, pu_.name], writes=[ac.name])
                else:
                    P.op("dve", lambda e, G_=G_, pu_=pu_: e.tensor_tensor(out=G_[:], in0=G_[:], in1=pu_[:], op=ALU.mult), reads=[G_.name, pu_.name], writes=[G_.name])
                    if n < 3:
                        P.op("pool", lambda e, ac=ac, G_=G_: e.tensor_tensor(out=ac[:], in0=ac[:], in1=G_[:], op=ALU.add), reads=[G_.name, ac.name], writes=[ac.name])
                    else:
                        P.op("pool", lambda e, ac=ac, G_=G_, dt=dt, sl=sl: e.tensor_tensor(out=pmx[:, dt, sl], in0=ac[:], in1=G_[:], op=ALU.add), reads=[G_.name, ac.name], writes=[pmx.name])
    P.emit()
    al = Bump(WS, REND)
    ostg = [al([128, 8, 128], F32, "ostg") for _ in range(2)]
    wo = al([128, 8, D], BF16, "wo")
    load_w_bf16(wo, dr["out_w"].rearrange("(k p) n -> p k n", p=128), D, ostg, wo.name)
    it = 0
    for ob in range(NOB):
        sl = slice(ob * 512, (ob + 1) * 512)
        for dt in range(8):
            pg_ = banks[it % 4]; it += 1
            for k in range(8):
                P.op("pe", lambda e, k=k, pg_=pg_, dt=dt, sl=sl: e.matmul(pg_[:], lhsT=wo[:, k, dt * 128:(dt + 1) * 128], rhs=pmx[:, k, sl], start=(k == 0), stop=(k == 7)),
                     reads=[wo.name, pmx.name], writes=[pg_.name])
            P.op("dve", lambda e, pg_=pg_, dt=dt, sl=sl: e.scalar_tensor_tensor(out=xo[:, dt, sl], in0=pg_[:], scalar=mods[:, 2, dt:dt + 1], in1=xo[:, dt, sl], op0=ALU.mult, op1=ALU.add),
                 reads=[pg_.name, mods.name, xo.name], writes=[xo.name])
    if dbg:
        P.dma("sp", lambda e: e.dma_start(out=dr["dbg_xmid"].rearrange("(k p) t -> p k t", p=128), in_=xo[:]), reads=[xo.name], writes=["dbg_xmid"])
        P.dma("pool", lambda e: e.dma_start(out=dr["dbg_br"].rearrange("n (k p) t -> p (n k) t", p=128), in_=br_own[:]), reads=[br_own.name], writes=["dbg_br"])
    P.emit()

    al = Bump(WS, REND)
    sq = al([128, 8, 512], F32, "sq"); h2f = al([128, 8, 512], F32, "h2f"); rstd = al([128, 512], F32, "rstd")
    al2 = Bump(R0 + 32 * KB, R2)
    combT = al2([NE, TO], BF16, "combT")
    sele = al2([NE, NE, 128], BF16, "sele")
    rw = al2([128, 8, NE], F32, "rw")
    rb = al2([128, NE], F32, "rb")
    P.dma("sp", lambda e: e.dma_start(out=rw[:], in_=dr["router_w"].rearrange("(k p) n -> p k n", p=128)), writes=[rw.name])
    P.dma("sp", lambda e: e.dma_start(out=rb[:], in_=dr["router_b"][0, :].partition_broadcast(128)), writes=[rb.name])
    P.op("pool", lambda e: e.memset(sele[:], 1.0), writes=[sele.name])
    P.op("pool", lambda e: e.affine_select(out=sele[:], in_=sele[:], pattern=[[-1, NE], [0, 128]], compare_op=ALU.is_equal, fill=0.0, base=0, channel_multiplier=1),
         reads=[sele.name], writes=[sele.name])
    prt = TT(banks[2][:, 0:NE], banks[2].name)
    pct = TT(banks[3][0:NE, 0:128], banks[3].name)
    rt = {n_: al2(sh, F32, "rt_" + n_) for n_, sh in (("sc", [128, NE]), ("bi", [128, NE]), ("m1", [128, 4]), ("t1", [128, NE]), ("m2", [128, 4]),
                                                      ("gs", [128, 4]), ("gm", [128, 1]), ("og", [128, 4]), ("mk", [128, NE]), ("o1", [128, NE]), ("o2", [128, NE]),
                                                      ("x1", [128, 1]), ("cb", [128, NE]), ("dn", [128, 1]))}
    MARK2 = al2.off

    def v3(t_):
        return t_[:].rearrange("p (g e) -> p g e", e=4)

    def bc3(t_):
        return t_[:].unsqueeze(2).to_broadcast([128, 4, 4])

    AX = mybir.AxisListType.X

    def dv(fn, reads, writes, extra=()):
        P.op("dve", fn, reads=[rt[r].name for r in reads] + list(extra), writes=[rt[w].name for w in writes])

    for ob in range(NOB):
        norm_block(xo[:, :, ob * 512:(ob + 1) * 512], xo.name, A2, mods[:, 3, :], sq, banks[ob % 2], rstd,
                   out_bf=hO[:, :, ob * 512:(ob + 1) * 512], okb=hO.name, out_f=h2f, okf=h2f.name)
        for tt in range(4):
            tok = ob * 4 + tt
            for k in range(8):
                P.op("pe", lambda e, k=k, tt=tt: e.matmul(prt[:], lhsT=h2f[:, k, tt * 128:(tt + 1) * 128], rhs=rw[:, k, :], start=(k == 0), stop=(k == 7)),
                     reads=[h2f.name, rw.name], writes=[prt.name])
            P.op("act", lambda e: e.activation(out=rt["sc"][:], in_=prt[:], func=AF.Sigmoid), reads=[prt.name], writes=[rt["sc"].name])
            dv(lambda e: e.tensor_tensor(out=rt["bi"][:], in0=rt["sc"][:], in1=rb[:], op=ALU.add), ["sc"], ["bi"], [rb.name])
            dv(lambda e: e.tensor_reduce(out=rt["m1"][:], in_=v3(rt["bi"]), axis=AX, op=ALU.max), ["bi"], ["m1"])
            dv(lambda e: e.tensor_tensor(out=v3(rt["t1"]), in0=v3(rt["bi"]), in1=bc3(rt["m1"]), op=ALU.is_equal), ["bi", "m1"], ["t1"])
            dv(lambda e: e.scalar_tensor_tensor(out=rt["t1"][:], in0=rt["t1"][:], scalar=-BIG, in1=rt["bi"][:], op0=ALU.mult, op1=ALU.add), ["t1", "bi"], ["t1"])
            dv(lambda e: e.tensor_reduce(out=rt["m2"][:], in_=v3(rt["t1"]), axis=AX, op=ALU.max), ["t1"], ["m2"])
            dv(lambda e: e.tensor_tensor(out=rt["gs"][:], in0=rt["m1"][:], in1=rt["m2"][:], op=ALU.add), ["m1", "m2"], ["gs"])
            dv(lambda e: e.tensor_reduce(out=rt["gm"][:], in_=rt["gs"][:], axis=AX, op=ALU.max), ["gs"], ["gm"])
            dv(lambda e: e.tensor_scalar(out=rt["og"][:], in0=rt["gs"][:], scalar1=rt["gm"][:, 0:1], scalar2=None, op0=ALU.is_equal), ["gs", "gm"], ["og"])
            dv(lambda e: e.tensor_tensor(out=v3(rt["mk"]), in0=v3(rt["bi"]), in1=bc3(rt["og"]), op=ALU.mult), ["bi", "og"], ["mk"])
            dv(lambda e: e.tensor_scalar(out=rt["og"][:], in0=rt["og"][:], scalar1=-1.0, scalar2=BIG, op0=ALU.add, op1=ALU.mult), ["og"], ["og"])
            dv(lambda e: e.tensor_tensor(out=v3(rt["mk"]), in0=v3(rt["mk"]), in1=bc3(rt["og"]), op=ALU.add), ["mk", "og"], ["mk"])
            dv(lambda e: e.tensor_reduce(out=rt["x1"][:], in_=rt["mk"][:], axis=AX, op=ALU.max), ["mk"], ["x1"])
            dv(lambda e: e.tensor_scalar(out=rt["o1"][:], in0=rt["mk"][:], scalar1=rt["x1"][:, 0:1], scalar2=None, op0=ALU.is_equal), ["mk", "x1"], ["o1"])
            dv(lambda e: e.scalar_tensor_tensor(out=rt["mk"][:], in0=rt["o1"][:], scalar=-BIG, in1=rt["mk"][:], op0=ALU.mult, op1=ALU.add), ["o1", "mk"], ["mk"])
            dv(lambda e: e.tensor_reduce(out=rt["x1"][:], in_=rt["mk"][:], axis=AX, op=ALU.max), ["mk"], ["x1"])
            dv(lambda e: e.tensor_scalar(out=rt["o2"][:], in0=rt["mk"][:], scalar1=rt["x1"][:, 0:1], scalar2=None, op0=ALU.is_equal), ["mk", "x1"], ["o2"])
            dv(lambda e: e.tensor_tensor(out=rt["o1"][:], in0=rt["o1"][:], in1=rt["o2"][:], op=ALU.add), ["o1", "o2"], ["o1"])
            dv(lambda e: e.tensor_tensor(out=rt["cb"][:], in0=rt["o1"][:], in1=rt["sc"][:], op=ALU.mult), ["o1", "sc"], ["cb"])
            dv(lambda e: e.tensor_reduce(out=rt["dn"][:], in_=rt["cb"][:], axis=AX, op=ALU.add), ["cb"], ["dn"])
            dv(lambda e: e.reciprocal(out=rt["dn"][:], in_=rt["dn"][:]), ["dn"], ["dn"])
            dv(lambda e: e.tensor_scalar(out=rt["cb"][:], in0=rt["cb"][:], scalar1=rt["dn"][:, 0:1], scalar2=None, op0=ALU.mult), ["cb", "dn"], ["cb"])
            P.op("pe", lambda e: e.transpose(out=pct[:], in_=rt["cb"][:], identity=ident_f[:]), reads=[rt["cb"].name, ident_f.name], writes=[pct.name])
            P.op("act", lambda e, tok=tok: e.copy(out=combT[:, tok * 128:(tok + 1) * 128], in_=pct[:]), reads=[pct.name], writes=[combT.name])
    P.emit()
    al = Bump(WS, REND)
    al2 = Bump(MARK2, R2)
    s13 = [al([128, 8, 128], F32, "s13") for _ in range(2)]
    s2 = [al([128, 4, 256], F32, "s2") for _ in range(2)]
    crep = [al([128, 512], F32, "crep") for _ in range(2)]
    sg = [al([128, 512], F32, "sg") for _ in range(2)]
    hm = [al([128, 4, 512], BF16, "hm") for _ in range(2)]
    w1b = [al2([128, 8, DE], BF16, "w1b") for _ in range(2)]
    w3b = [al2([128, 8, DE], BF16, "w3b") for _ in range(2)]
    w2b = [al2([128, 4, D], BF16, "w2b") for _ in range(2)]
    pgm = banks[0:2]; pum = banks[2:4]; pym = banks[4:6]; pcr = banks[6]
    it = 0; iy = 0; ib = 0
    for ex in range(NE):
        w1_, w3_, w2_ = w1b[ex % 2], w3b[ex % 2], w2b[ex % 2]
        load_w_bf16(w1_, dr["exp_w1"][ex].rearrange("(k p) n -> p k n", p=128), DE, s13, w1_.name, q="sp")
        load_w_bf16(w3_, dr["exp_w3"][ex].rearrange("(k p) n -> p k n", p=128), DE, s13, w3_.name, q="sp")
        load_w_bf16(w2_, dr["exp_w2"][ex].rearrange("(k p) n -> p k n", p=128), D, s2, w2_.name, q="pool")
        for ob in range(NOB):
            sl = slice(ob * 512, (ob + 1) * 512)
            cr = crep[ib % 2]; hm_ = hm[ib % 2]; ib += 1
            P.op("pe", lambda e, ex=ex, sl=sl: e.matmul(pcr[:], lhsT=sele[:, ex, :], rhs=combT[:, sl], start=True, stop=True), reads=[sele.name, combT.name], writes=[pcr.name])
            P.op("act", lambda e, cr=cr: e.copy(out=cr[:], in_=pcr[:]), reads=[pcr.name], writes=[cr.name])
            for ft in range(4):
                pg_, pu_, sg_ = pgm[it % 2], pum[it % 2], sg[it % 2]; it += 1
                for k in range(8):
                    P.op("pe", lambda e, k=k, pg_=pg_, w1_=w1_, ft=ft, sl=sl: e.matmul(pg_[:], lhsT=w1_[:, k, ft * 128:(ft + 1) * 128], rhs=hO[:, k, sl], start=(k == 0), stop=(k == 7)),
                         reads=[w1_.name, hO.name], writes=[pg_.name])
                for k in range(8):
                    P.op("pe", lambda e, k=k, pu_=pu_, w3_=w3_, ft=ft, sl=sl: e.matmul(pu_[:], lhsT=w3_[:, k, ft * 128:(ft + 1) * 128], rhs=hO[:, k, sl], start=(k == 0), stop=(k == 7)),
                         reads=[w3_.name, hO.name], writes=[pu_.name])
                P.op("act", lambda e, pg_=pg_, sg_=sg_: e.activation(out=sg_[:], in_=pg_[:], func=AF.Silu), reads=[pg_.name], writes=[sg_.name])
                P.op("dve", lambda e, pu_=pu_, sg_=sg_: e.tensor_tensor(out=sg_[:], in0=sg_[:], in1=pu_[:], op=ALU.mult), reads=[pu_.name, sg_.name], writes=[sg_.name])
                P.op("pool", lambda e, sg_=sg_, cr=cr, hm_=hm_, ft=ft: e.tensor_tensor(out=hm_[:, ft, :], in0=sg_[:], in1=cr[:], op=ALU.mult), reads=[sg_.name, cr.name], writes=[hm_.name])
            for dt in range(8):
                py_ = pym[iy % 2]; iy += 1
                for ft in range(4):
                    P.op("pe", lambda e, ft=ft, py_=py_, w2_=w2_, hm_=hm_, dt=dt: e.matmul(py_[:], lhsT=w2_[:, ft, dt * 128:(dt + 1) * 128], rhs=hm_[:, ft, :], start=(ft == 0), stop=(ft == 3)),
                         reads=[w2_.name, hm_.name], writes=[py_.name])
                P.op("dve", lambda e, py_=py_, dt=dt, sl=sl: e.scalar_tensor_tensor(out=xo[:, dt, sl], in0=py_[:], scalar=mods[:, 5, dt:dt + 1], in1=xo[:, dt, sl], op0=ALU.mult, op1=ALU.add),
                     reads=[py_.name, mods.name, xo.name], writes=[xo.name])
    P.emit()

    outv = dr["outT"].rearrange("(k p) t -> p k t", p=128)
    if last:
        al = Bump(WS, REND)
        sq = al([128, 8, 512], F32, "sq"); rstd = al([128, 512], F32, "rstd")
        al2 = Bump(R0 + 32 * KB, R2)
        of = [al2([128, 8, 512], F32, "of") for _ in range(2)]
        for ob in range(NOB):
            o_ = of[ob % 2]
            norm_block(xo[:, :, ob * 512:(ob + 1) * 512], xo.name, gf, zero8, sq, banks[ob % 2], rstd, out_f=o_, okf=o_.name)
            P.dma("sp", lambda e, o_=o_, ob=ob: e.dma_start(out=outv[:, :, ob * 512:(ob + 1) * 512], in_=o_[:]), reads=[o_.name], writes=["outT"])
    else:
        for k in range(8):
            P.dma("sp", lambda e, k=k: e.dma_start(out=outv[:, k, :], in_=xo[:, k, :]), reads=[xo.name], writes=["outT"])
    keys = ["outT"] + (["dbg_xmid", "dbg_br"] if dbg else [])
    P.wait_all("sp", keys)
    P.emit()
    top.close()
    return nc


def _fm(v, nt):
    return np.ascontiguousarray(np.asarray(v, np.float32).reshape(nt, 128).T)


def core_inputs(inp, l, b, j, xb):
    T = xb.shape[0]
    f = np.float32
    m = {}
    m["xT"] = np.ascontiguousarray(xb.T, dtype=f)
    xo = xb.reshape(T // 256, 2, 128, D)[:, j].reshape(T // 2, D)
    m["xoT"] = np.ascontiguousarray(xo.T, dtype=f)
    m["flags"] = np.ascontiguousarray(np.tile(np.array([1.0 - j, float(j)], f), (128, 1)))
    m["cT"] = _fm(inp["c"][b], 8)
    m["pos"] = np.ascontiguousarray(np.asarray(inp["positions"][b][:T], np.int32).reshape(1, T))
    m["ada_w"] = np.ascontiguousarray(inp["ada_w"][l], dtype=f)
    m["ada_bT"] = np.ascontiguousarray(np.asarray(inp["ada_b"][l], f).reshape(6, 8, 128).transpose(2, 0, 1).reshape(128, 48))
    m["g1T"] = _fm(inp["mix_norm_g"][l], 8)
    m["g2T"] = _fm(inp["ffn_norm_g"][l], 8)
    m["gfT"] = _fm(inp["final_norm_g"], 8)
    m["w_in"] = np.ascontiguousarray(inp["w_in"][l], dtype=f)
    m["gate_w"] = np.ascontiguousarray(inp["gate_w"][l], dtype=f)
    m["gate_bT"] = np.ascontiguousarray(np.asarray(inp["gate_b"][l], f).reshape(4, 8, 128).transpose(2, 0, 1).reshape(128, 32))
    m["branch_w"] = np.ascontiguousarray(inp["branch_w"][l], dtype=f)
    m["out_w"] = np.ascontiguousarray(inp["out_w"][l], dtype=f)
    m["rg_conv_wT"] = np.ascontiguousarray(np.asarray(inp["rg_conv_w"][l], f).reshape(4, 2, 128).transpose(2, 1, 0))
    m["rg_conv_bT"] = _fm(inp["rg_conv_b"][l], 2)
    m["rg_wr"] = np.ascontiguousarray(inp["rg_wr"][l], dtype=f)
    m["rg_wi"] = np.ascontiguousarray(inp["rg_wi"][l], dtype=f)
    m["rg_brT"] = _fm(np.asarray(inp["rg_br"][l]).reshape(-1), 2)
    m["rg_biT"] = _fm(np.asarray(inp["rg_bi"][l]).reshape(-1), 2)
    m["rg_lamT"] = _fm(inp["rg_lam"][l], 2)
    m["s5_lreT"] = _fm(np.asarray(inp["s5_lam_re"][l]).reshape(-1), 8)
    m["s5_limT"] = _fm(np.asarray(inp["s5_lam_im"][l]).reshape(-1), 8)
    m["s5_lstT"] = _fm(np.repeat(np.asarray(inp["s5_log_step"][l], f), 64), 8)
    m["s5_dT"] = _fm(inp["s5_d"][l], 2)
    m["s5_glbT"] = _fm(inp["s5_glu_b"][l], 2)
    m["s5_glu_w"] = np.ascontiguousarray(inp["s5_glu_w"][l], dtype=f)
    for nm, src, isb in (("s5_brT", "s5_b_re", True), ("s5_biT", "s5_b_im", True), ("s5_crT", "s5_c_re", False), ("s5_ciT", "s5_c_im", False)):
        a = np.asarray(inp[src][l], f)
        o = np.zeros((8, 128, 128), f)
        for s_ in range(8):
            for gg in range(2):
                c0 = 32 * (s_ % 4) + 16 * gg
                if isb:
                    o[s_, c0:c0 + 16, gg * 64:(gg + 1) * 64] = a[2 * s_ + gg].T
                else:
                    o[s_, gg * 64:(gg + 1) * 64, c0:c0 + 16] = a[2 * s_ + gg].T
        m[nm] = o
    p_ = np.arange(128)
    rc = np.zeros((128, 4), f)
    rc[:, 0] = (10000.0 ** (-(p_ % 32) / 32.0)) / (2.0 * math.pi)
    rc[:, 1] = np.where((p_ % 64) < 32, -1.0, 1.0)
    rc[:, 2] = (10000.0 ** (-(p_ % 16) / 16.0)) / (2.0 * math.pi)
    rc[:, 3] = np.where((p_ % 32) < 16, -1.0, 1.0)
    m["rope_c"] = rc
    pm64 = np.zeros((128, 128), f); pm32 = np.zeros((128, 128), f)
    for c_ in range(128):
        pm64[(c_ + 32) if (c_ % 64) < 32 else (c_ - 32), c_] = 1.0
        pm32[(c_ + 16) if (c_ % 32) < 16 else (c_ - 16), c_] = 1.0
    m["perm64"] = pm64; m["perm32"] = pm32
    m["w_ik4"] = np.ascontiguousarray(np.tile(np.asarray(inp["w_in"][l][:, C_IK:C_IK + 32], f), (1, 4)))
    up = np.zeros((128, 3, 256), f)
    up[0:32, 0] = np.asarray(inp["rw_w_up"][l], f); up[32:64, 1] = np.asarray(inp["rw_a_up"][l], f); up[64:128, 2] = np.asarray(inp["rw_g_up"][l], f)
    m["rw_upT"] = up
    prm = np.zeros((128, 7, 2), f)
    for i_, nm in enumerate(("rw_w0", "rw_a0", "rw_k_k", "rw_k_a", "rw_gn_w", "rw_gn_b", "rw_r_k")):
        prm[:, i_, :] = _fm(np.asarray(inp[nm][l]).reshape(-1), 2)
    m["rw_prmT"] = prm
    m["rw_muT"] = _fm(inp["rw_mu"][l], 7)
    pp_, qq_ = np.meshgrid(np.arange(128), np.arange(128), indexing="ij")
    cst = np.zeros((128, 6, 128), f)
    cst[:, 0] = (pp_ // 64 == qq_ // 64); cst[:, 1] = (pp_ < qq_); cst[:, 2] = (pp_ <= qq_); cst[:, 3] = (pp_ > qq_)
    cst[:, 4] = (qq_ < 64); cst[:, 5] = (qq_ >= 64)
    m["rw_cst"] = cst
    hm_ = np.zeros((128, 2), f); hm_[0:64, 0] = 1.0; hm_[64:128, 1] = 1.0
    m["rw_hmask"] = hm_
    m["router_w"] = np.ascontiguousarray(inp["router_w"], dtype=f)
    m["router_b"] = np.ascontiguousarray(np.asarray(inp["router_b"], f).reshape(1, NE))
    m["exp_w1"] = np.ascontiguousarray(inp["exp_w1"][l], dtype=f)
    m["exp_w3"] = np.ascontiguousarray(inp["exp_w3"][l], dtype=f)
    m["exp_w2"] = np.ascontiguousarray(inp["exp_w2"][l], dtype=f)
    return m


def assemble(outs, T):
    nb = len(outs) // 2
    res = np.zeros((nb, T // 256, 2, 128, D), np.float32)
    for b in range(nb):
        for j in range(2):
            res[b, :, j] = outs[2 * b + j].T.reshape(T // 256, 128, D)
    return res.reshape(nb, T, D)


_NC_CACHE = {}


def kernel(**inp):
    inp = {k: np.asarray(v) for k, v in inp.items()}
    x = np.asarray(inp["x"], np.float32)
    B, T, _ = x.shape
    L = inp["ada_w"].shape[0]
    for l in range(L):
        last = (l == L - 1)
        key = (T, last)
        if key not in _NC_CACHE:
            _NC_CACHE[key] = build_layer(T, last)
        nc = _NC_CACHE[key]
        in_maps = [core_inputs(inp, l, b, j, x[b]) for b in range(B) for j in range(2)]
        res = run_bass_kernel_spmd(nc, in_maps, core_ids=list(range(2 * B)))
        x = assemble([r["outT"] for r in res.results], T)
    return x
```
